# Optimizing a Trainium2 kernel written in Bass

```python
import jax
import jax.numpy as jnp
from jax import lax
import numpy as np

D_MODEL = 1024
BATCH = 8
SEQ = 8192
DEPTH = 1

MEM_LEN = 256
SB_HEAD_DIM = 64
SB_WIDTH = D_MODEL // 2
SB_HEADS = SB_WIDTH // SB_HEAD_DIM
Q_BLOCK = 128
LRU_WIDTH = D_MODEL // 2
LRU_BLOCKS = 8
LRU_BLOCK_DIM = LRU_WIDTH // LRU_BLOCKS
CONV_WIDTH = 4
LRU_C = 8.0
X_HEADS = 4
X_WIDTH = D_MODEL // 2
X_HEAD_DIM = X_WIDTH // X_HEADS
N_BRANCH = 3
IN_COLS = 3 * SB_WIDTH + 2 * LRU_WIDTH + X_WIDTH + N_BRANCH * D_MODEL
N_GROUPS = 4
EXPERTS_PER_GROUP = 8
TOP_K = 2
EXPERT_FF = D_MODEL // 4
EPS = 1e-6

kernel_name = 'hybrid_sb_rglru_memx_hmoe_layer'


def rms_norm(x, g):
    xf = x.astype(jnp.float32)
    y = xf * lax.rsqrt(jnp.mean(xf * xf, axis=-1, keepdims=True) + EPS)
    return (y * g.astype(jnp.float32)).astype(x.dtype)


def stick_breaking_attention(q, k, v):
    B, S, H, Dh = q.shape
    n_blk = S // Q_BLOCK
    scale = Dh ** -0.5
    key_pos = jnp.arange(S)
    q_blocks = q.reshape(B, n_blk, Q_BLOCK, H, Dh).transpose(1, 0, 2, 3, 4)

    def one_block(args):
        qb, blk = args
        z = jnp.einsum('bqhd,bkhd->bhqk', qb, k, preferred_element_type=jnp.float32) * scale
        q_pos = blk * Q_BLOCK + jnp.arange(Q_BLOCK)
        causal = key_pos[None, :] < q_pos[:, None]
        sp = jax.nn.softplus(z)
        log_1m = jnp.where(causal, -sp, 0.0)
        rev = lax.cumsum(log_1m, axis=3, reverse=True)
        between = jnp.concatenate([rev[..., 1:], jnp.zeros_like(rev[..., :1])], axis=-1)
        weights = jnp.where(causal, jnp.exp(z - sp + between), 0.0)
        return jnp.einsum('bhqk,bkhd->bqhd', weights.astype(v.dtype), v)

    out = lax.map(one_block, (q_blocks, jnp.arange(n_blk)))
    return out.transpose(1, 0, 2, 3, 4).reshape(B, S, H * Dh)


def _linear_recurrence_combine(left, right):
    a_l, b_l = left
    a_r, b_r = right
    return (a_l * a_r, a_r * b_l + b_r)


def rglru_branch(x_in, y_in, conv_w, conv_b, w_a, b_a, w_i, b_i, lam):
    B, S, C = x_in.shape
    xc = lax.conv_general_dilated(
        x_in, conv_w[:, None, :], window_strides=(1,), padding=[(CONV_WIDTH - 1, 0)],
        dimension_numbers=('NWC', 'WIO', 'NWC'), feature_group_count=C) + conv_b
    xb = xc.reshape(B, S, LRU_BLOCKS, LRU_BLOCK_DIM)
    r = jax.nn.sigmoid(jnp.einsum('bsni,nij->bsnj', xb, w_a).reshape(B, S, C) + b_a)
    i = jax.nn.sigmoid(jnp.einsum('bsni,nij->bsnj', xb, w_i).reshape(B, S, C) + b_i)
    log_a = (-LRU_C * r.astype(jnp.float32)) * jax.nn.softplus(-lam.astype(jnp.float32))
    a = jnp.exp(log_a)
    b = jnp.sqrt(-jnp.expm1(2.0 * log_a)) * (i * xc).astype(jnp.float32)
    _, h = lax.associative_scan(_linear_recurrence_combine, (a, b), axis=1)
    return h.astype(x_in.dtype) * jax.nn.gelu(y_in)


def memory_cross_attention(q_in, mem, g_mem, w_mem_kv, g_q, g_k):
    B, S, _ = q_in.shape
    M = mem.shape[1]
    kv = jnp.einsum('bmd,dc->bmc', rms_norm(mem, g_mem), w_mem_kv)
    k, v = jnp.split(kv, 2, axis=-1)
    q = rms_norm(q_in.reshape(B, S, X_HEADS, X_HEAD_DIM), g_q)
    k = rms_norm(k.reshape(B, M, X_HEADS, X_HEAD_DIM), g_k)
    v = v.reshape(B, M, X_HEADS, X_HEAD_DIM)
    s = jnp.einsum('bshd,bmhd->bhsm', q, k, preferred_element_type=jnp.float32) * (X_HEAD_DIM ** -0.5)
    p = jax.nn.softmax(s, axis=-1)
    o = jnp.einsum('bhsm,bmhd->bshd', p.astype(v.dtype), v)
    return o.reshape(B, S, X_WIDTH)


def hierarchical_moe(h, w_group, b_group, w_expert, b_expert, w_gate, w_up, w_down):
    B, S, D = h.shape
    T = B * S
    t = h.reshape(T, D)
    group_logits = jnp.einsum('td,dg->tg', t, w_group).astype(jnp.float32) + b_group
    group_probs = jax.nn.softmax(group_logits, axis=-1)
    g_prob, g_idx = lax.top_k(group_probs, 1)
    expert_logits = (jnp.einsum('td,de->te', t, w_expert).astype(jnp.float32) + b_expert)
    expert_logits = expert_logits.reshape(T, N_GROUPS, EXPERTS_PER_GROUP)
    in_group = expert_logits[jnp.arange(T), g_idx[:, 0]]
    top_vals, top_idx = lax.top_k(in_group, TOP_K)
    top_w = jax.nn.softmax(top_vals, axis=-1)
    expert_w = jnp.sum(top_w[..., None] * jax.nn.one_hot(top_idx, EXPERTS_PER_GROUP, dtype=jnp.float32), axis=1)
    combine = (jax.nn.one_hot(g_idx[:, 0], N_GROUPS, dtype=jnp.float32)[:, :, None]
               * (g_prob * expert_w)[:, None, :]).astype(h.dtype)
    out = jnp.zeros_like(t)
    for g in range(N_GROUPS):
        gate = jnp.einsum('td,edf->tef', t, w_gate[g])
        up = jnp.einsum('td,edf->tef', t, w_up[g])
        act = jax.nn.silu(gate) * up * combine[:, g, :, None]
        out = out + jnp.einsum('tef,efd->td', act, w_down[g])
    return out.reshape(B, S, D)


def hybrid_layer(x, mem, g_mix, w_in, g_q_sb, g_k_sb, conv_w, conv_b, lru_w_a, lru_b_a,
                 lru_w_i, lru_b_i, lru_lambda, g_mem, w_mem_kv, g_q_x, g_k_x, w_branch, w_out,
                 g_ffn, w_group, b_group, w_expert, b_expert, w_gate, w_up, w_down):
    B, S, D = x.shape
    h = rms_norm(x, g_mix)
    proj = jnp.einsum('bsd,dc->bsc', h, w_in)
    splits = [SB_WIDTH, 2 * SB_WIDTH, 3 * SB_WIDTH, 3 * SB_WIDTH + LRU_WIDTH,
              3 * SB_WIDTH + 2 * LRU_WIDTH, 3 * SB_WIDTH + 2 * LRU_WIDTH + X_WIDTH]
    q_sb, k_sb, v_sb, x_lru, y_lru, q_x, gate_logits = jnp.split(proj, splits, axis=-1)

    q_sb = rms_norm(q_sb.reshape(B, S, SB_HEADS, SB_HEAD_DIM), g_q_sb)
    k_sb = rms_norm(k_sb.reshape(B, S, SB_HEADS, SB_HEAD_DIM), g_k_sb)
    v_sb = v_sb.reshape(B, S, SB_HEADS, SB_HEAD_DIM)
    o_sb = stick_breaking_attention(q_sb, k_sb, v_sb)
    o_lru = rglru_branch(x_lru, y_lru, conv_w, conv_b, lru_w_a, lru_b_a, lru_w_i, lru_b_i, lru_lambda)
    o_x = memory_cross_attention(q_x, mem, g_mem, w_mem_kv, g_q_x, g_k_x)

    branches = jnp.stack([o_sb, o_lru, o_x], axis=2)
    u = jnp.einsum('bsnc,ncd->bsnd', branches, w_branch)
    gates = jax.nn.sigmoid(gate_logits.reshape(B, S, N_BRANCH, D))
    merged = jnp.sum(gates * u, axis=2)
    x = x + jnp.einsum('bsd,de->bse', merged, w_out)

    x = x + hierarchical_moe(rms_norm(x, g_ffn), w_group, b_group, w_expert, b_expert,
                             w_gate, w_up, w_down)
    return x


def setup_inputs(seed: int = 0) -> dict:
    key = jax.random.key(seed)
    ks = jax.random.split(key, 32)
    f32 = jnp.float32

    def nrm(k, shape, scale):
        return jax.random.normal(k, shape, f32) * scale

    def gain(k, n):
        return 1.0 + 0.01 * jax.random.normal(k, (DEPTH, n), f32)

    u = jax.random.uniform(ks[12], (DEPTH, LRU_WIDTH), f32, 0.9, 0.999)
    s = u ** (1.0 / LRU_C)
    lru_lambda = jnp.log(s) - jnp.log1p(-s)

    return {
        'x': nrm(ks[0], (BATCH, SEQ, D_MODEL), 1.0),
        'mem': nrm(ks[1], (BATCH, MEM_LEN, D_MODEL), 1.0),
        'g_mix': gain(ks[2], D_MODEL),
        'w_in': nrm(ks[3], (DEPTH, D_MODEL, IN_COLS), D_MODEL ** -0.5),
        'g_q_sb': gain(ks[4], SB_HEAD_DIM),
        'g_k_sb': gain(ks[5], SB_HEAD_DIM),
        'conv_w': nrm(ks[6], (DEPTH, CONV_WIDTH, LRU_WIDTH), CONV_WIDTH ** -0.5),
        'conv_b': nrm(ks[7], (DEPTH, LRU_WIDTH), 0.01),
        'lru_w_a': nrm(ks[8], (DEPTH, LRU_BLOCKS, LRU_BLOCK_DIM, LRU_BLOCK_DIM), LRU_BLOCK_DIM ** -0.5),
        'lru_b_a': nrm(ks[9], (DEPTH, LRU_WIDTH), 0.01),
        'lru_w_i': nrm(ks[10], (DEPTH, LRU_BLOCKS, LRU_BLOCK_DIM, LRU_BLOCK_DIM), LRU_BLOCK_DIM ** -0.5),
        'lru_b_i': nrm(ks[11], (DEPTH, LRU_WIDTH), 0.01),
        'lru_lambda': lru_lambda,
        'g_mem': gain(ks[13], D_MODEL),
        'w_mem_kv': nrm(ks[14], (DEPTH, D_MODEL, 2 * X_WIDTH), D_MODEL ** -0.5),
        'g_q_x': gain(ks[15], X_HEAD_DIM),
        'g_k_x': gain(ks[16], X_HEAD_DIM),
        'w_branch': nrm(ks[17], (DEPTH, N_BRANCH, SB_WIDTH, D_MODEL), SB_WIDTH ** -0.5),
        'w_out': nrm(ks[18], (DEPTH, D_MODEL, D_MODEL), D_MODEL ** -0.5),
        'g_ffn': gain(ks[19], D_MODEL),
        'w_group': nrm(ks[20], (DEPTH, D_MODEL, N_GROUPS), D_MODEL ** -0.5),
        'b_group': nrm(ks[21], (DEPTH, N_GROUPS), 0.01),
        'w_expert': nrm(ks[22], (DEPTH, D_MODEL, N_GROUPS * EXPERTS_PER_GROUP), D_MODEL ** -0.5),
        'b_expert': nrm(ks[23], (DEPTH, N_GROUPS * EXPERTS_PER_GROUP), 0.01),
        'w_gate': nrm(ks[24], (DEPTH, N_GROUPS, EXPERTS_PER_GROUP, D_MODEL, EXPERT_FF), D_MODEL ** -0.5),
        'w_up': nrm(ks[25], (DEPTH, N_GROUPS, EXPERTS_PER_GROUP, D_MODEL, EXPERT_FF), D_MODEL ** -0.5),
        'w_down': nrm(ks[26], (DEPTH, N_GROUPS, EXPERTS_PER_GROUP, EXPERT_FF, D_MODEL), EXPERT_FF ** -0.5),
    }


def reference(x, mem, g_mix, w_in, g_q_sb, g_k_sb, conv_w, conv_b, lru_w_a, lru_b_a,
              lru_w_i, lru_b_i, lru_lambda, g_mem, w_mem_kv, g_q_x, g_k_x, w_branch, w_out,
              g_ffn, w_group, b_group, w_expert, b_expert, w_gate, w_up, w_down):
    for l in range(DEPTH):
        x = hybrid_layer(x, mem, g_mix[l], w_in[l], g_q_sb[l], g_k_sb[l], conv_w[l], conv_b[l],
                         lru_w_a[l], lru_b_a[l], lru_w_i[l], lru_b_i[l], lru_lambda[l], g_mem[l],
                         w_mem_kv[l], g_q_x[l], g_k_x[l], w_branch[l], w_out[l], g_ffn[l],
                         w_group[l], b_group[l], w_expert[l], b_expert[l], w_gate[l], w_up[l],
                         w_down[l])
    return x
```

```python
import contextlib
from contextlib import ExitStack
import numpy as np
import concourse.bass as bass
import concourse.mybir as mybir
from concourse.bass_utils import run_bass_kernel_spmd

F32 = mybir.dt.float32
BF16 = mybir.dt.bfloat16
AF = mybir.ActivationFunctionType
ALU = mybir.AluOpType
AX = mybir.AxisListType

SEM_CH = 30000
SYNC_SAME = True
N_DMA_SEMS = 6
EPS = 1e-6
BIG = 1.0e9


class Buf:
    __slots__ = ("name", "writes", "reads")

    def __init__(self, name=""):
        self.name = name
        self.writes = []
        self.reads = []


class TT:
    __slots__ = ("t", "b")

    def __init__(self, t, name=""):
        self.t = t
        self.b = Buf(name)


class Rot:
    def __init__(self, items):
        self.items = items
        self.i = 0

    def next(self):
        x = self.items[self.i % len(self.items)]
        self.i += 1
        return x


class SemPool:
    def __init__(self, nc, n):
        self.h = [nc.alloc_semaphore(name=f"gsem{i}") for i in range(n)]
        self.i = 0

    def take(self):
        h = self.h[self.i]
        self.i += 1
        return h

    def reset(self, nc):
        hs = self.h[:self.i]
        with nc.Block() as block:
            def body(g):
                for h in hs:
                    g.sem_clear(h)
            block.gpsimd(body)
        self.i = 0


class Sched:
    ENGS = ("pe", "act", "dve", "pool", "sp")

    def __init__(self, nc, sync_same_engine=True):
        self.nc = nc
        self.sempool = nc._sempool
        self.streams = {e: [] for e in self.ENGS}
        self.count = {e: 0 for e in self.ENGS}
        self.dma_count = {}
        self.sync_same = sync_same_engine
        self.dma_rr = {e: 0 for e in self.ENGS}
        self.waited = {}

    def _deps_for(self, reads, writes, par=False):
        deps = []
        for b in reads:
            deps.extend(b.writes)
        for b in writes:
            if not par:
                deps.extend(b.writes)
            deps.extend(b.reads)
        best = {}
        for d in deps:
            k = (d[0], d[1])
            if k not in best or best[k][2] < d[2]:
                best[k] = d
        return list(best.values())

    def _emit(self, eng, fn, deps, is_dma, dma_q=None):
        waits = []
        for d in deps:
            if d is None:
                continue
            if d[0] == "E":
                _, e2, n = d
                if e2 == eng and (eng == "pe" or (not self.sync_same and eng != "pool")) and not is_dma:
                    continue
                key = (eng, "E", e2)
            else:
                key = (eng, "D", d[1])
                n = d[2]
            if self.waited.get(key, 0) >= n:
                continue
            self.waited[key] = n
            waits.append(d)
        if is_dma:
            k = dma_q
            self.dma_count[k] = self.dma_count.get(k, 0) + 1
            tok = ("D", k, self.dma_count[k] * 16)
        else:
            self.count[eng] += 1
            tok = ("E", eng, self.count[eng])
        self.streams[eng].append((fn, waits, tok))
        return tok

    def _post(self, tok, reads, writes, par=False):
        for b in reads:
            b.reads.append(tok)
        for b in writes:
            if par and not b.reads:
                b.writes.append(tok)
            else:
                b.writes = [tok]
            b.reads = []

    def op(self, eng, fn, reads=(), writes=()):
        reads = [r.b if isinstance(r, TT) else r for r in reads]
        writes = [w.b if isinstance(w, TT) else w for w in writes]
        tok = self._emit(eng, fn, self._deps_for(reads, writes), False)
        self._post(tok, reads, writes)
        return tok

    def dma(self, eng, out, in_, reads=(), writes=(), par=False):
        reads = [r.b if isinstance(r, TT) else r for r in reads]
        writes = [w.b if isinstance(w, TT) else w for w in writes]
        deps = self._deps_for(reads, writes, par)
        base = {"sp": 0, "pool": N_DMA_SEMS, "act": 2 * N_DMA_SEMS}[eng]
        k = base + self.dma_rr[eng]
        self.dma_rr[eng] = (self.dma_rr[eng] + 1) % N_DMA_SEMS
        prev = self.dma_count.get(k, 0)
        if prev > 0:
            deps.append(("D", k, prev * 16))
        tok = self._emit(eng, lambda e, o=out, i=in_: e.dma_start(out=o, in_=i), deps, True, dma_q=k)
        self._post(tok, reads, writes, par)
        return tok

    def finalize(self, final_eng="sp"):
        nc = self.nc
        fin_waits = [("D", k, v * 16) for k, v in self.dma_count.items()]
        with ExitStack() as st:
            esems = {}
            for e in self.ENGS:
                n = (self.count[e] // SEM_CH) + 1
                esems[e] = [self.sempool.take() for i in range(n)]
            dsems = {k: self.sempool.take() for k in sorted(self.dma_count)}
            block = st.enter_context(nc.Block())

            def sem_for(tok):
                if tok[0] == "E":
                    _, e2, n = tok
                    idx = (n - 1) // SEM_CH
                    return esems[e2][idx], n - idx * SEM_CH
                return dsems[tok[1]], tok[2]

            def run(engname):
                def body(eng):
                    for fn, waits, tok in self.streams[engname]:
                        for w in waits:
                            s, v = sem_for(w)
                            eng.wait_ge(s, v)
                        ins = fn(eng)
                        s, v = sem_for(tok)
                        ins.then_inc(s, 1 if tok[0] == "E" else 16)
                    if engname == final_eng:
                        for w in fin_waits:
                            s, v = sem_for(w)
                            eng.wait_ge(s, v)
                return body

            block.tensor(run("pe"))
            block.scalar(run("act"))
            block.vector(run("dve"))
            block.gpsimd(run("pool"))
            block.sync(run("sp"))


NVEC = 8 * 3 + 4 + 16 + 4 * 4


def build(S, debug=False, phases=(0, 1, 2, 3, 4), n_exp=32):
    assert S % 512 == 0
    NT = S // 512
    NB = S // 128
    nc = bass.Bass("TRN2", target_bir_lowering=False)
    nc._sempool = SemPool(nc, 96)

    def din(name, shape):
        return nc.dram_tensor(name, shape, F32, kind="ExternalInput").ap()

    x_d = din("x", [S, 1024])
    mem_d = din("mem", [256, 1024])
    w_in_d = din("w_in", [1024, 6144])
    vecs_d = din("vecs", [128, NVEC])
    gfull_d = din("gfull", [128, 2, 1024])
    gmemfull_d = din("gmemfull", [128, 1024])
    rbias_d = din("rbias", [128, 36])
    wabd_d = din("wabd", [128, 8, 128])
    wkv_d = din("w_mem_kv", [1024, 1024])
    wbr_d = din("w_branch", [3, 512, 1024])
    wout_d = din("w_out", [1024, 1024])
    wr_d = din("w_router", [1024, 36])
    wg_d = din("w_gate", [32, 1024, 256])
    wu_d = din("w_up", [32, 1024, 256])
    wd_d = din("w_down", [32, 256, 1024])
    consts_d = din("consts", [128, 8, 128])
    out_d = nc.dram_tensor("out", [S, 1024], F32, kind="ExternalOutput").ap()

    skind = "ExternalOutput" if debug else "Internal"

    def dscr(name, shape, dt):
        return nc.dram_tensor(name, shape, dt, kind=skind).ap()

    qT_d = dscr("qT", [512, S], BF16)
    kT_d = dscr("kT", [512, S], BF16)
    v_d = dscr("v", [S, 512], BF16)
    olruT_d = dscr("olruT", [512, S], BF16)
    oxT_d = dscr("oxT", [512, S], BF16)
    gT_d = dscr("gT", [3072, S], BF16)
    osbT_d = dscr("osbT", [512, S], BF16)
    x1_d = dscr("x1", [S, 1024], F32)
    h2T_d = dscr("h2T", [1024, S], BF16)
    comb_d = dscr("comb", [S, 32], F32)

    V_GQSB, V_GKSB, V_GQX, V_GKX = 0, 1, 2, 3
    V_CONVW = 4
    V_CONVB = 20
    V_BA, V_BI, V_LAM = 24, 28, 32
    V_GFFN = 36

    PH = phases

    def mk(st):
        def sb(name, shape, dt=F32):
            return TT(st.enter_context(nc.sbuf_tensor("sb_" + name, shape, dt)), name)

        def ps(name, dt=F32, n=512):
            return TT(st.enter_context(nc.psum_tensor("ps_" + name, [128, n], dt)), name)
        return sb, ps

    def make_norm_helpers(SX, junk, sc, cst, wk16, wk32, PA):
        def rms_rstd(src_tt, src_ap, sstile, col, nfeat):
            SX.op("act", lambda e: e.activation(out=junk.t[:, 0:nfeat], in_=src_ap, func=AF.Square, accum_out=sstile.t[:, col:col + 1]),
                  reads=[src_tt], writes=[junk, sstile])
            SX.op("act", lambda e: e.activation(out=sstile.t[:, col:col + 1], in_=sstile.t[:, col:col + 1], func=AF.Ln, scale=1.0 / nfeat, bias=epsb.t[:, 0:1]),
                  reads=[sstile, epsb], writes=[sstile])
            SX.op("act", lambda e: e.activation(out=sstile.t[:, col:col + 1], in_=sstile.t[:, col:col + 1], func=AF.Exp, scale=-0.5),
                  reads=[sstile], writes=[sstile])

        def feat_norm_a(P, ncols):
            sq = wk16.next()
            SX.op("act", lambda e: e.activation(out=sq.t[:, 0:ncols], in_=P.t[:, 0:ncols], func=AF.Square), reads=[P], writes=[sq])
            return sq

        def feat_norm_b(P, sq, ncols, onesmat, nfeat, gcol, out_tt, out16):
            A = PA.next()
            SX.op("pe", lambda e: e.matmul(A.t[:, 0:ncols], onesmat, sq.t[:, 0:ncols], start=True, stop=True), reads=[cst, sq], writes=[A])
            ln = wk32.next()
            SX.op("act", lambda e: e.activation(out=ln.t[:, 0:ncols], in_=A.t[:, 0:ncols], func=AF.Ln, scale=1.0 / nfeat, bias=epsb.t[:, 0:1]), reads=[A, epsb], writes=[ln])
            r = wk32.next()
            SX.op("act", lambda e: e.activation(out=r.t[:, 0:ncols], in_=ln.t[:, 0:ncols], func=AF.Exp, scale=-0.5), reads=[ln], writes=[r])
            SX.op("dve", lambda e: e.scalar_tensor_tensor(out=out16, in0=P.t[:, 0:ncols], scalar=sc.t[:, gcol:gcol + 1], in1=r.t[:, 0:ncols],
                                                         op0=ALU.mult, op1=ALU.mult), reads=[P, sc, r], writes=[out_tt])

        def feat_norm(P, ncols, onesmat, nfeat, gcol, out_tt, out16):
            sq = feat_norm_a(P, ncols)
            feat_norm_b(P, sq, ncols, onesmat, nfeat, gcol, out_tt, out16)
        feat_norm.a = feat_norm_a
        feat_norm.b = feat_norm_b
        return rms_rstd, feat_norm

    with ExitStack() as stO:
        sbO, psO = mk(stO)
        vecs = sbO("vecs", [128, NVEC])
        cst = sbO("cst", [128, 8, 128], BF16)
        kmem = sbO("kmem", [128, 4, 256], BF16)
        vmem = sbO("vmem", [128, 2, 512], BF16)
        sc = sbO("sc", [128, 16])
        epsb = sbO("epsb", [128, 1])
        junk = sbO("junk", [128, 1024])
        ident = cst.t[:, 0, :]
        ones64 = cst.t[:, 4, :]
        ones128 = cst.t[:, 5, :]

        if 0 in PH:
          with ExitStack() as st:
            S0 = Sched(nc, sync_same_engine=SYNC_SAME)
            sb, ps = mk(st)
            gmemf = sb("gmemf", [128, 1024])
            wkv = sb("wkv", [128, 8, 1024], BF16)
            memT = sb("memT", [128, 8, 256], BF16)
            xpool = Rot([sb(f"xm{i}", [128, 1024]) for i in range(2)])
            hnpool = Rot([sb(f"hm{i}", [128, 1024], BF16) for i in range(2)])
            ssm = sb("ssm", [128, 4])
            wk32 = Rot([sb(f"wk0_{i}", [128, 512]) for i in range(4)])
            wk16 = Rot([sb(f"wb0_{i}", [128, 512], BF16) for i in range(2)])
            PT = Rot([ps(f"PT0_{i}", BF16, 1024) for i in range(2)])
            PM = Rot([ps(f"PM0_{i}") for i in range(4)])
            PA = Rot([ps(f"PA0_{i}") for i in range(2)])
            rms_rstd, feat_norm = make_norm_helpers(S0, junk, sc, cst, wk16, wk32, PA)

            S0.dma("sp", vecs.t[:], vecs_d, writes=[vecs])
            S0.dma("sp", gmemf.t[:], gmemfull_d, writes=[gmemf])
            S0.dma("pool", cst.t[:], consts_d, writes=[cst])
            for c in range(8):
                S0.dma("pool", wkv.t[:, c, :], wkv_d[c * 128:(c + 1) * 128, :], writes=[wkv], par=True)
            S0.op("pool", lambda e: e.memset(epsb.t[:], EPS), writes=[epsb])
            S0.op("dve", lambda e: e.tensor_scalar(out=sc.t[:, 0:1], in0=vecs.t[:, V_GQSB:V_GQSB + 1], scalar1=0.125, scalar2=None, op0=ALU.mult),
                  reads=[vecs], writes=[sc])
            S0.op("dve", lambda e: e.tensor_copy(out=sc.t[:, 1:2], in_=vecs.t[:, V_GKSB:V_GKSB + 1]), reads=[vecs], writes=[sc])
            S0.op("dve", lambda e: e.tensor_scalar(out=sc.t[:, 2:3], in0=vecs.t[:, V_GQX:V_GQX + 1], scalar1=float(128 ** -0.5), scalar2=None, op0=ALU.mult),
                  reads=[vecs], writes=[sc])
            S0.op("dve", lambda e: e.tensor_copy(out=sc.t[:, 3:4], in_=vecs.t[:, V_GKX:V_GKX + 1]), reads=[vecs], writes=[sc])
            S0.op("act", lambda e: e.activation(out=sc.t[:, 12:16], in_=vecs.t[:, V_LAM:V_LAM + 4], func=AF.Exp, scale=-1.0), reads=[vecs], writes=[sc])
            S0.op("act", lambda e: e.activation(out=sc.t[:, 12:16], in_=sc.t[:, 12:16], func=AF.Ln, bias=1.0), reads=[sc], writes=[sc])
            S0.op("dve", lambda e: e.tensor_scalar(out=sc.t[:, 4:8], in0=sc.t[:, 12:16], scalar1=-8.0, scalar2=None, op0=ALU.mult), reads=[sc], writes=[sc])
            S0.op("dve", lambda e: e.tensor_scalar(out=sc.t[:, 8:12], in0=sc.t[:, 12:16], scalar1=-16.0, scalar2=None, op0=ALU.mult), reads=[sc], writes=[sc])

            def p0_blk(blk):
                xt = xpool.next()
                S0.dma("sp", xt.t[:], mem_d[blk * 128:(blk + 1) * 128, :], writes=[xt])
                rms_rstd(xt, xt.t[:], ssm, blk, 1024)
                hn = hnpool.next()
                S0.op("dve", lambda e: e.scalar_tensor_tensor(out=hn.t[:], in0=xt.t[:], scalar=ssm.t[:, blk:blk + 1], in1=gmemf.t[:],
                                                             op0=ALU.mult, op1=ALU.mult), reads=[xt, ssm, gmemf], writes=[hn])
                pt = PT.next()
                for c in range(8):
                    S0.op("pe", lambda e, c=c: e.transpose(pt.t[:, c * 128:(c + 1) * 128], hn.t[:, c * 128:(c + 1) * 128], ident),
                          reads=[hn, cst], writes=[pt])
                S0.op("dve", lambda e: e.tensor_copy(out=memT.t[:, :, blk * 128:(blk + 1) * 128],
                                                     in_=pt.t[:].rearrange("p (c n) -> p c n", c=8)), reads=[pt], writes=[memT])
            for blk in range(2):
                p0_blk(blk)

            def p0_k(h):
                P = PM.next()
                for k in range(8):
                    S0.op("pe", lambda e, k=k: e.matmul(P.t[:, 0:256], wkv.t[:, k, h * 128:(h + 1) * 128], memT.t[:, k, :], start=(k == 0), stop=(k == 7)),
                          reads=[wkv, memT], writes=[P])
                feat_norm(P, 256, ones128, 128, 3, kmem, kmem.t[:, h, :])
            for h in range(4):
                p0_k(h)

            def p0_v(blk):
                P = PM.next()
                for k in range(8):
                    S0.op("pe", lambda e, k=k: e.matmul(P.t[:], memT.t[:, k, blk * 128:(blk + 1) * 128], wkv.t[:, k, 512:1024], start=(k == 0), stop=(k == 7)),
                          reads=[wkv, memT], writes=[P])
                S0.op("act", lambda e: e.activation(out=vmem.t[:, blk, :], in_=P.t[:], func=AF.Copy), reads=[P], writes=[vmem])
            for blk in range(2):
                p0_v(blk)
            S0.finalize()
            for t in (vecs, cst, kmem, vmem, sc, epsb, junk):
                t.b.writes = []
                t.b.reads = []

        if 1 in PH:
          with ExitStack() as st:
            S1 = Sched(nc, sync_same_engine=SYNC_SAME)
            sb, ps = mk(st)
            win = sb("win", [128, 8, 6144], BF16)
            gmixf = sb("gmixf", [128, 1024])
            wabd = sb("wabd", [128, 8, 128], BF16)
            xbuf = [sb(f"xbuf{c}", [128, 515]) for c in range(4)]
            state = sb("state", [128, 4])
            hT = Rot([sb(f"hT{i}", [128, 8, 512], BF16) for i in range(2)])
            xpool = Rot([sb(f"xt{i}", [128, 1024]) for i in range(3)])
            hnpool = Rot([sb(f"hn{i}", [128, 1024], BF16) for i in range(2)])
            sspool = Rot([sb(f"ss{i}", [128, 4]) for i in range(3)])
            wk32 = Rot([sb(f"wk{i}", [128, 512]) for i in range(10)])
            wk16 = Rot([sb(f"wb{i}", [128, 512], BF16) for i in range(6)])
            ob16 = Rot([sb(f"ob{i}", [128, 512], BF16) for i in range(6)])
            PT = Rot([ps(f"PT{i}", BF16, 1024) for i in range(2)])
            PM = Rot([ps(f"PM{i}") for i in range(4)])
            PA = Rot([ps(f"PA{i}") for i in range(2)])
            rms_rstd, feat_norm = make_norm_helpers(S1, junk, sc, cst, wk16, wk32, PA)

            S1.dma("sp", gmixf.t[:], gfull_d[:, 0, :], writes=[gmixf])
            S1.dma("pool", wabd.t[:], wabd_d, writes=[wabd])
            for c in range(8):
                for hf in range(2):
                    S1.dma("pool", win.t[:, c, hf * 3072:(hf + 1) * 3072], w_in_d[c * 128:(c + 1) * 128, hf * 3072:(hf + 1) * 3072], writes=[win], par=True)
            for c in range(4):
                S1.op("pool", lambda e, c=c: e.memset(xbuf[c].t[:, 0:3], 0.0), writes=[xbuf[c]])
            S1.op("pool", lambda e: e.memset(state.t[:], 0.0), writes=[state])

            def p1_xblk(it, blk, hTt, sst):
                r0 = it * 512 + blk * 128
                xt = xpool.next()
                S1.dma("sp", xt.t[:], x_d[r0:r0 + 128, :], writes=[xt])
                rms_rstd(xt, xt.t[:], sst, blk, 1024)
                hn = hnpool.next()
                S1.op("dve", lambda e: e.scalar_tensor_tensor(out=hn.t[:], in0=xt.t[:], scalar=sst.t[:, blk:blk + 1], in1=gmixf.t[:],
                                                             op0=ALU.mult, op1=ALU.mult), reads=[xt, sst, gmixf], writes=[hn])
                pt = PT.next()
                for c in range(8):
                    S1.op("pe", lambda e, c=c: e.transpose(pt.t[:, c * 128:(c + 1) * 128], hn.t[:, c * 128:(c + 1) * 128], ident),
                          reads=[hn, cst], writes=[pt])
                S1.op("dve", lambda e: e.tensor_copy(out=hTt.t[:, :, blk * 128:(blk + 1) * 128],
                                                     in_=pt.t[:].rearrange("p (c n) -> p c n", c=8)), reads=[pt], writes=[hTt])

            def proj_fm(ci, hTt):
                P = PM.next()
                for k in range(8):
                    S1.op("pe", lambda e, k=k: e.matmul(P.t[:], win.t[:, k, ci * 128:(ci + 1) * 128], hTt.t[:, k, :], start=(k == 0), stop=(k == 7)),
                          reads=[win, hTt], writes=[P])
                return P

            def p1_qk(ci, hTt, c0):
                L = {}

                def a():
                    L["P"] = proj_fm(ci, hTt)
                    L["sq"] = feat_norm.a(L["P"], 512)

                def b():
                    o = ob16.next()
                    feat_norm.b(L["P"], L["sq"], 512, ones64, 64, 0 if ci < 4 else 1, o, o.t[:])
                    dst = (qT_d if ci < 4 else kT_d)[(ci % 4) * 128:(ci % 4 + 1) * 128, c0:c0 + 512]
                    S1.dma("sp", dst, o.t[:], reads=[o])
                return [a, b]

            def p1_v(blk, hTt, c0):
                P = PM.next()
                for k in range(8):
                    S1.op("pe", lambda e, k=k: e.matmul(P.t[:], hTt.t[:, k, blk * 128:(blk + 1) * 128], win.t[:, k, 1024:1536], start=(k == 0), stop=(k == 7)),
                          reads=[win, hTt], writes=[P])
                o = ob16.next()
                S1.op("act", lambda e: e.activation(out=o.t[:], in_=P.t[:], func=AF.Copy), reads=[P], writes=[o])
                S1.dma("sp", v_d[c0 + blk * 128:c0 + (blk + 1) * 128, :], o.t[:], reads=[o])

            def p1_qx(h, hTt, c0):
                L = {}

                def a():
                    L["P"] = proj_fm(20 + h, hTt)
                    L["sq"] = feat_norm.a(L["P"], 512)

                def a2():
                    qn = wk16.next()
                    L["qn"] = qn
                    feat_norm.b(L["P"], L["sq"], 512, ones128, 128, 2, qn, qn.t[:])

                def b():
                    qn = L["qn"]
                    pms = []

                    def sx(mb):
                        Sx = PM.next()
                        S1.op("pe", lambda e: e.matmul(Sx.t[:], kmem.t[:, h, mb * 128:(mb + 1) * 128], qn.t[:], start=True, stop=True),
                              reads=[kmem, qn], writes=[Sx])
                        pm = wk16.next()
                        S1.op("act", lambda e: e.activation(out=pm.t[:], in_=Sx.t[:], func=AF.Exp), reads=[Sx], writes=[pm])
                        pms.append(pm)
                    for mb in range(2):
                        sx(mb)
                    L["pms"] = pms

                def c():
                    pms = L["pms"]
                    Ox = PM.next()
                    Dn = PA.next()
                    for mb in range(2):
                        S1.op("pe", lambda e, mb=mb: e.matmul(Ox.t[:], vmem.t[:, mb, h * 128:(h + 1) * 128], pms[mb].t[:], start=(mb == 0), stop=(mb == 1)),
                              reads=[vmem, pms[mb]], writes=[Ox])
                    for mb in range(2):
                        S1.op("pe", lambda e, mb=mb: e.matmul(Dn.t[:], ones128, pms[mb].t[:], start=(mb == 0), stop=(mb == 1)),
                              reads=[cst, pms[mb]], writes=[Dn])
                    rd = wk32.next()
                    S1.op("dve", lambda e: e.reciprocal(out=rd.t[:], in_=Dn.t[:]), reads=[Dn], writes=[rd])
                    o = ob16.next()
                    S1.op("dve", lambda e: e.tensor_tensor(out=o.t[:], in0=Ox.t[:], in1=rd.t[:], op=ALU.mult), reads=[Ox, rd], writes=[o])
                    S1.dma("sp", oxT_d[h * 128:(h + 1) * 128, c0:c0 + 512], o.t[:], reads=[o])
                return [a, a2, b, c]

            def p1_gate(j, hTt, c0):
                P = proj_fm(24 + j, hTt)
                o = ob16.next()
                S1.op("act", lambda e: e.activation(out=o.t[:], in_=P.t[:], func=AF.Sigmoid), reads=[P], writes=[o])
                S1.dma("sp", gT_d[j * 128:(j + 1) * 128, c0:c0 + 512], o.t[:], reads=[o])

            def lru_stages(c, hTt, c0):
                L = {}
                xb = xbuf[c]
                cw = V_CONVW + c * 4

                def s0():
                    Px = proj_fm(12 + c, hTt)
                    S1.op("act", lambda e: e.activation(out=xb.t[:, 3:515], in_=Px.t[:], func=AF.Copy), reads=[Px], writes=[xb])
                    Py = proj_fm(16 + c, hTt)
                    gy = wk32.next()
                    S1.op("act", lambda e: e.activation(out=gy.t[:], in_=Py.t[:], func=AF.Gelu_apprx_tanh), reads=[Py], writes=[gy])
                    L["gy"] = gy

                def s1():
                    xc = wk32.next()
                    L["xc"] = xc
                    S1.op("dve", lambda e: e.tensor_scalar(out=xc.t[:], in0=xb.t[:, 0:512], scalar1=vecs.t[:, cw:cw + 1],
                                                          scalar2=vecs.t[:, V_CONVB + c:V_CONVB + c + 1], op0=ALU.mult, op1=ALU.add),
                          reads=[xb, vecs], writes=[xc])
                    for k in range(1, 4):
                        S1.op("dve", lambda e, k=k: e.scalar_tensor_tensor(out=xc.t[:], in0=xb.t[:, k:k + 512], scalar=vecs.t[:, cw + k:cw + k + 1],
                                                                          in1=xc.t[:], op0=ALU.mult, op1=ALU.add),
                              reads=[xb, vecs, xc], writes=[xc])
                    S1.op("dve", lambda e: e.tensor_copy(out=xb.t[:, 0:3], in_=xb.t[:, 512:515]), reads=[xb], writes=[xb])
                    xcb = wk16.next()
                    L["xcb"] = xcb
                    S1.op("act", lambda e: e.activation(out=xcb.t[:], in_=xc.t[:], func=AF.Copy), reads=[xc], writes=[xcb])

                def s2():
                    xcb = L["xcb"]
                    Pr = PA.next()
                    S1.op("pe", lambda e: e.matmul(Pr.t[:], wabd.t[:, c, :], xcb.t[:], start=True, stop=True), reads=[wabd, xcb], writes=[Pr])
                    Pi = PA.next()
                    S1.op("pe", lambda e: e.matmul(Pi.t[:], wabd.t[:, 4 + c, :], xcb.t[:], start=True, stop=True), reads=[wabd, xcb], writes=[Pi])
                    rr = wk32.next()
                    S1.op("act", lambda e: e.activation(out=rr.t[:], in_=Pr.t[:], func=AF.Sigmoid, bias=vecs.t[:, V_BA + c:V_BA + c + 1]),
                          reads=[Pr, vecs], writes=[rr])
                    ii = wk32.next()
                    S1.op("act", lambda e: e.activation(out=ii.t[:], in_=Pi.t[:], func=AF.Sigmoid, bias=vecs.t[:, V_BI + c:V_BI + c + 1]),
                          reads=[Pi, vecs], writes=[ii])
                    L["rr"], L["ii"] = rr, ii

                def s3():
                    rr = L["rr"]
                    aa = wk32.next()
                    S1.op("act", lambda e: e.activation(out=aa.t[:], in_=rr.t[:], func=AF.Exp, scale=sc.t[:, 4 + c:5 + c]), reads=[rr, sc], writes=[aa])
                    a2 = wk32.next()
                    S1.op("act", lambda e: e.activation(out=a2.t[:], in_=rr.t[:], func=AF.Exp, scale=sc.t[:, 8 + c:9 + c]), reads=[rr, sc], writes=[a2])
                    S1.op("act", lambda e: e.activation(out=a2.t[:], in_=a2.t[:], func=AF.Ln, scale=-1.0, bias=1.0), reads=[a2], writes=[a2])
                    S1.op("act", lambda e: e.activation(out=a2.t[:], in_=a2.t[:], func=AF.Exp, scale=0.5), reads=[a2], writes=[a2])
                    L["aa"], L["a2"] = aa, a2

                def s4():
                    ii, xc, a2, aa = L["ii"], L["xc"], L["a2"], L["aa"]
                    S1.op("dve", lambda e: e.tensor_tensor(out=ii.t[:], in0=ii.t[:], in1=xc.t[:], op=ALU.mult), reads=[ii, xc], writes=[ii])
                    S1.op("dve", lambda e: e.tensor_tensor(out=ii.t[:], in0=ii.t[:], in1=a2.t[:], op=ALU.mult), reads=[ii, a2], writes=[ii])
                    hs = wk32.next()
                    L["hs"] = hs
                    S1.op("dve", lambda e: e.tensor_tensor_scan(out=hs.t[:], data0=aa.t[:], data1=ii.t[:], initial=state.t[:, c:c + 1],
                                                               op0=ALU.mult, op1=ALU.add), reads=[aa, ii, state], writes=[hs])
                    S1.op("dve", lambda e: e.tensor_copy(out=state.t[:, c:c + 1], in_=hs.t[:, 511:512]), reads=[hs], writes=[state])

                def s5():
                    hs, gy = L["hs"], L["gy"]
                    o = ob16.next()
                    S1.op("dve", lambda e: e.tensor_tensor(out=o.t[:], in0=hs.t[:], in1=gy.t[:], op=ALU.mult), reads=[hs, gy], writes=[o])
                    S1.dma("sp", olruT_d[c * 128:(c + 1) * 128, c0:c0 + 512], o.t[:], reads=[o])
                return [s0, s1, s2, s3, s4, s5]

            def p1_front(it):
                hTt = hT.next()
                sst = sspool.next()
                for blk in range(4):
                    p1_xblk(it, blk, hTt, sst)
                return hTt

            hT_next = p1_front(0)
            for it in range(NT):
                c0 = it * 512
                hTt = hT_next
                items = []
                for i4 in range(4):
                    items.append(p1_qx(i4, hTt, c0))
                    items.append(p1_qk(2 * i4, hTt, c0))
                    items.append(p1_qk(2 * i4 + 1, hTt, c0))
                    items.append([lambda blk=i4: p1_v(blk, hTt, c0)])
                active = []
                while items or active:
                    while items and len(active) < 3:
                        active.append(items.pop(0))
                    for itm in list(active):
                        itm.pop(0)()
                        if not itm:
                            active.remove(itm)
                stages = []
                for j in range(24):
                    if j % 6 == 0:
                        stages = lru_stages(j // 6, hTt, c0)
                    p1_gate(j, hTt, c0)
                    stages[j % 6]()
                    if j == 9 and it + 1 < NT:
                        hT_next = p1_front(it + 1)
            S1.finalize()

    if 2 in PH:
      with ExitStack() as st:
        S2 = Sched(nc, sync_same_engine=SYNC_SAME)
        sb, ps = mk(st)
        KT = sb("KT", [128, 4, S], BF16)
        VV = sb("VV", [128, NB, 512], BF16)
        cst = sb("cst2", [128, 8, 128], BF16)
        qpool = Rot([sb(f"qt{i}", [128, 512], BF16) for i in range(3)])
        upool = Rot([sb(f"ue{i}", [128, 2, 512]) for i in range(3)])
        sppool = Rot([sb(f"sp{i}", [128, 2, 512], BF16) for i in range(4)])
        wpool = Rot([sb(f"ww{i}", [128, 2, 512], BF16) for i in range(4)])
        rspool = Rot([sb(f"rs{i}", [128, 2, 512], BF16) for i in range(4)])
        opool = Rot([sb(f"oo{i}", [64, 2, 512], BF16) for i in range(2)])
        PP = Rot([ps(f"PP{i}", F32, 1024) for i in range(3)])
        PO = [ps(f"PO{i}") for i in range(2)]
        negLI = cst.t[:, 1, :]
        negOnes = cst.t[:, 2, :]
        mask2 = cst.t[:, 6:8, :]

        S2.dma("pool", cst.t[:], consts_d, writes=[cst])
        for hp in range(4):
            S2.dma("sp", KT.t[:, hp, :], kT_d[hp * 128:(hp + 1) * 128, :], writes=[KT], par=True)
        for kb in range(NB):
            S2.dma("sp", VV.t[:, kb, :], v_d[kb * 128:(kb + 1) * 128, :], writes=[VV], par=True)

        units = []
        for g in range(NT):
            for hp in range(4):
                kbs = list(range(4 * g + 3, -1, -1))
                for idx, kb in enumerate(kbs):
                    units.append(dict(g=g, hp=hp, kb=kb, first=(idx == 0), last=(idx == len(kbs) - 1)))
        grp = {}

        def v3(t, lo, hi=512):
            return t.t[:].rearrange("p (h n) -> p h n", h=2)[:, :, lo:hi]

        def stA(u):
            g, hp, kb = u["g"], u["hp"], u["kb"]
            if u["first"]:
                q = qpool.next()
                S2.dma("sp", q.t[:], qT_d[hp * 128:(hp + 1) * 128, g * 512:(g + 1) * 512], writes=[q])
                rsa = rspool.next()
                rsb = rspool.next()
                S2.op("pool", lambda e: e.memset(rsa.t[:], 0.0), writes=[rsa])
                S2.op("pool", lambda e: e.memset(rsb.t[:], 0.0), writes=[rsb])
                grp[(g, hp)] = dict(q=q, rs=[rsa, rsb], n=0)
            G = grp[(g, hp)]
            u["G"] = G
            i = kb - 4 * g
            lo = max(i, 0) * 128
            u["lo"] = lo
            u["diag"] = i >= 0
            P = PP.next()
            u["P"] = P
            q = G["q"]
            for hh in range(2):
                p0, p1 = hh * 64, (hh + 1) * 64
                S2.op("pe", lambda e, hh=hh, p0=p0, p1=p1: e.matmul(P.t[:, hh * 512 + lo:(hh + 1) * 512], KT.t[p0:p1, hp, kb * 128:(kb + 1) * 128], q.t[p0:p1, lo:512],
                                                                    start=True, stop=True), reads=[KT, q], writes=[P])

        def stB1(u):
            P, lo = u["P"], u["lo"]
            ue = upool.next()
            u["ue"] = ue
            S2.op("act", lambda e: e.activation(out=ue.t[:, :, lo:512], in_=v3(P, lo), func=AF.Exp), reads=[P], writes=[ue])

        def stB2(u):
            lo, ue = u["lo"], u["ue"]
            sp = sppool.next()
            u["sp"] = sp
            S2.op("act", lambda e: e.activation(out=sp.t[:, :, lo:512], in_=ue.t[:, :, lo:512], func=AF.Ln, bias=1.0), reads=[ue], writes=[sp])
            if u["diag"]:
                S2.op("pool", lambda e: e.tensor_tensor(out=sp.t[:, :, lo:lo + 128], in0=sp.t[:, :, lo:lo + 128], in1=mask2, op=ALU.mult),
                      reads=[sp, cst], writes=[sp])

        def stC(u):
            P, lo, sp, G = u["P"], u["lo"], u["sp"], u["G"]
            rs = G["rs"][G["n"] % 2]
            rsn = G["rs"][(G["n"] + 1) % 2]
            G["n"] += 1
            first, last = u["first"], u["last"]
            for hh in range(2):
                S2.op("pe", lambda e, hh=hh: e.matmul(P.t[:, hh * 512 + lo:(hh + 1) * 512], negLI, sp.t[:, hh, lo:512], start=False, stop=first, skip_group_check=True),
                      reads=[cst, sp], writes=[P])
                if not first:
                    S2.op("pe", lambda e, hh=hh: e.matmul(P.t[:, hh * 512 + lo:(hh + 1) * 512], negOnes, rs.t[:, hh, lo:512], start=False, stop=True, skip_group_check=True),
                          reads=[cst, rs], writes=[P])
            if not last:
                S2.op("pool", lambda e: e.tensor_tensor(out=rsn.t[:, :, lo:512], in0=rs.t[:, :, lo:512], in1=sp.t[:, :, lo:512], op=ALU.add),
                      reads=[rs, sp], writes=[rsn])

        def stD(u):
            P, lo = u["P"], u["lo"]
            w = wpool.next()
            u["w"] = w
            S2.op("act", lambda e: e.activation(out=w.t[:, :, lo:512], in_=v3(P, lo), func=AF.Exp), reads=[P], writes=[w])
            if u["diag"]:
                S2.op("dve", lambda e: e.tensor_tensor(out=w.t[:, :, lo:lo + 128], in0=w.t[:, :, lo:lo + 128], in1=mask2, op=ALU.mult),
                      reads=[w, cst], writes=[w])

        def stE(u):
            w, lo, hp, kb, g = u["w"], u["lo"], u["hp"], u["kb"], u["g"]
            kw = dict(start=True, stop=True) if u["first"] else dict(start=False, stop=True, skip_group_check=True)
            for hh in range(2):
                h = hp * 2 + hh
                O = PO[hh]
                S2.op("pe", lambda e, O=O, hh=hh, h=h: e.matmul(O.t[0:64, lo:512], VV.t[:, kb, h * 64:(h + 1) * 64], w.t[:, hh, lo:512], **kw),
                      reads=[VV, w], writes=[O])
            if u["last"]:
                o = opool.next()
                for hh in range(2):
                    S2.op("dve", lambda e, hh=hh: e.tensor_copy(out=o.t[:, hh, :], in_=PO[hh].t[0:64, :]), reads=[PO[hh]], writes=[o])
                for hh in range(2):
                    h = hp * 2 + hh
                    S2.dma("sp", osbT_d[h * 64:(h + 1) * 64, g * 512:(g + 1) * 512], o.t[:, hh, :], reads=[o])

        nU = len(units)
        for s in range(nU + 3):
            if s < nU:
                stA(units[s])
            if 0 <= s - 1 < nU:
                stB1(units[s - 1])
                stB2(units[s - 1])
                stC(units[s - 1])
            if 0 <= s - 2 < nU:
                stD(units[s - 2])
            if 0 <= s - 3 < nU:
                stE(units[s - 3])
        S2.finalize()

    if 3 in PH:
      with ExitStack() as st:
        S3 = Sched(nc, sync_same_engine=SYNC_SAME)
        sb, ps = mk(st)
        wbr = sb("wbr", [128, 12, 1024], BF16)
        wout = sb("wout", [128, 8, 1024], BF16)
        wr32 = sb("wr32", [128, 8, 36])
        rbias = sb("rbias", [128, 36])
        cstf = sb("cstf", [128, 128])
        epsb = sb("epsb3", [128, 1])
        gtp = Rot([sb(f"gt{i}", [128, 3, 512], BF16) for i in range(10)])
        onp = Rot([sb(f"on{i}", [128, 12, 512], BF16) for i in range(2)])
        mTp = Rot([sb(f"mT{i}", [128, 8, 512], BF16) for i in range(2)])
        xpool = Rot([sb(f"x3_{i}", [128, 1024]) for i in range(3)])
        x1pool = Rot([sb(f"x1_{i}", [128, 1024]) for i in range(2)])
        h2pool = Rot([sb(f"h2_{i}", [128, 1024]) for i in range(3)])
        h2T32p = Rot([sb(f"h2T32_{i}", [128, 8, 128]) for i in range(3)])
        h2Tb = Rot([sb(f"h2Tb{i}", [128, 8, 512], BF16) for i in range(2)])
        junk = sb("junk3", [128, 1024])
        sspool = Rot([sb(f"ss3_{i}", [128, 4]) for i in range(3)])
        wk32 = Rot([sb(f"wk3_{i}", [128, 512]) for i in range(6)])
        rt = Rot([sb(f"rt{i}", [128, 256]) for i in range(3)])
        cbp = Rot([sb(f"cb{i}", [128, 32]) for i in range(3)])
        PM = Rot([ps(f"PM3_{i}") for i in range(4)])
        PTf = Rot([ps(f"PT3_{i}") for i in range(2)])
        PLp = Rot([ps(f"PL3_{i}") for i in range(2)])

        S3.op("pool", lambda e: e.memset(epsb.t[:], EPS), writes=[epsb])
        vecs3 = sb("vecs3", [128, NVEC])
        S3.dma("sp", vecs3.t[:], vecs_d, writes=[vecs3])
        S3.dma("sp", rbias.t[:], rbias_d, writes=[rbias])
        S3.dma("sp", cstf.t[:], consts_d[:, 0, :], writes=[cstf])
        for c in range(8):
            S3.dma("sp", wr32.t[:, c, :], wr_d[c * 128:(c + 1) * 128, :], writes=[wr32], par=True)
        for n in range(3):
            for c in range(4):
                S3.dma("pool", wbr.t[:, n * 4 + c, :], wbr_d[n, c * 128:(c + 1) * 128, :], writes=[wbr], par=True)
        for c in range(8):
            S3.dma("pool", wout.t[:, c, :], wout_d[c * 128:(c + 1) * 128, :], writes=[wout], par=True)

        def p3_merge(j, on, mT, gt):
            ms = []

            def one(n):
                U = PM.next()
                for c in range(4):
                    S3.op("pe", lambda e, c=c: e.matmul(U.t[:], wbr.t[:, n * 4 + c, j * 128:(j + 1) * 128], on.t[:, n * 4 + c, :], start=(c == 0), stop=(c == 3)),
                          reads=[wbr, on], writes=[U])
                m = wk32.next()
                S3.op("dve", lambda e: e.tensor_tensor(out=m.t[:], in0=U.t[:], in1=gt.t[:, n, :], op=ALU.mult), reads=[U, gt], writes=[m])
                ms.append(m)
            for n in range(3):
                one(n)
            a, b, c_ = ms
            S3.op("pool", lambda e: e.tensor_tensor(out=a.t[:], in0=a.t[:], in1=b.t[:], op=ALU.add), reads=[a, b], writes=[a])
            S3.op("pool", lambda e: e.tensor_tensor(out=mT.t[:, j, :], in0=a.t[:], in1=c_.t[:], op=ALU.add), reads=[a, c_], writes=[mT])

        def p3_route(blk, r0, PL):
            R = rt.next()
            lg = R.t[:, 0:36]

            def dv(fn, eng="dve"):
                S3.op(eng, fn, reads=[R], writes=[R])
            S3.op("dve", lambda e: e.tensor_tensor(out=lg, in0=PL.t[:, 0:36], in1=rbias.t[:], op=ALU.add), reads=[PL, rbias], writes=[R])
            gmax, ngmax, gsum, gprob = R.t[:, 40:41], R.t[:, 41:42], R.t[:, 42:43], R.t[:, 43:44]
            ge, goh, pen = R.t[:, 44:48], R.t[:, 48:52], R.t[:, 52:56]
            elm, oh1, elm2, oh2 = R.t[:, 64:96], R.t[:, 96:128], R.t[:, 128:160], R.t[:, 160:192]
            m1, m2, dd, ed, den, w1, w2, cw1, cw2 = [R.t[:, 200 + i:201 + i] for i in range(9)]
            t1 = R.t[:, 216:248]
            dv(lambda e: e.reduce_max(out=gmax, in_=R.t[:, 0:4], axis=AX.X))
            dv(lambda e: e.tensor_scalar(out=ngmax, in0=gmax, scalar1=-1.0, scalar2=None, op0=ALU.mult))
            dv(lambda e: e.activation(out=ge, in_=R.t[:, 0:4], func=AF.Exp, bias=ngmax, accum_out=gsum), eng="act")
            dv(lambda e: e.reciprocal(out=gprob, in_=gsum))
            dv(lambda e: e.tensor_scalar(out=goh, in0=R.t[:, 0:4], scalar1=gmax, scalar2=None, op0=ALU.is_equal))
            dv(lambda e: e.tensor_scalar(out=pen, in0=goh, scalar1=1.0, scalar2=BIG, op0=ALU.subtract, op1=ALU.mult))
            yield
            for gg in range(4):
                dv(lambda e, gg=gg: e.tensor_scalar(out=R.t[:, 64 + gg * 8:64 + (gg + 1) * 8], in0=R.t[:, 4 + gg * 8:4 + (gg + 1) * 8],
                                                    scalar1=R.t[:, 52 + gg:53 + gg], scalar2=None, op0=ALU.add))
            dv(lambda e: e.reduce_max(out=m1, in_=elm, axis=AX.X))
            dv(lambda e: e.tensor_scalar(out=oh1, in0=elm, scalar1=m1, scalar2=None, op0=ALU.is_equal))
            dv(lambda e: e.scalar_tensor_tensor(out=elm2, in0=oh1, scalar=-BIG, in1=elm, op0=ALU.mult, op1=ALU.add))
            yield
            dv(lambda e: e.reduce_max(out=m2, in_=elm2, axis=AX.X))
            dv(lambda e: e.tensor_scalar(out=oh2, in0=elm2, scalar1=m2, scalar2=None, op0=ALU.is_equal))
            dv(lambda e: e.tensor_tensor(out=dd, in0=m2, in1=m1, op=ALU.subtract))
            dv(lambda e: e.activation(out=ed, in_=dd, func=AF.Exp), eng="act")
            dv(lambda e: e.tensor_scalar(out=den, in0=ed, scalar1=1.0, scalar2=None, op0=ALU.add))
            dv(lambda e: e.reciprocal(out=w1, in_=den))
            yield
            dv(lambda e: e.tensor_tensor(out=w2, in0=ed, in1=w1, op=ALU.mult))
            dv(lambda e: e.tensor_tensor(out=cw1, in0=w1, in1=gprob, op=ALU.mult))
            dv(lambda e: e.tensor_tensor(out=cw2, in0=w2, in1=gprob, op=ALU.mult))
            dv(lambda e: e.tensor_scalar(out=t1, in0=oh1, scalar1=cw1, scalar2=None, op0=ALU.mult))
            cb = cbp.next()
            S3.op("dve", lambda e: e.scalar_tensor_tensor(out=cb.t[:], in0=oh2, scalar=cw2, in1=t1, op0=ALU.mult, op1=ALU.add), reads=[R], writes=[cb])
            S3.dma("sp", comb_d[r0:r0 + 128, :], cb.t[:], reads=[cb])

        def p3_xload(it, blk):
            r0 = it * 512 + blk * 128
            xt = xpool.next()
            S3.dma("sp", xt.t[:], x_d[r0:r0 + 128, :], writes=[xt])
            return xt

        def p3_outproj(it, blk, mT, sst, xt):
            r0 = it * 512 + blk * 128
            x1 = x1pool.next()

            def yhalf(half):
                Y = PM.next()
                for k in range(8):
                    S3.op("pe", lambda e, k=k: e.matmul(Y.t[:], mT.t[:, k, blk * 128:(blk + 1) * 128], wout.t[:, k, half * 512:(half + 1) * 512],
                                                        start=(k == 0), stop=(k == 7)), reads=[mT, wout], writes=[Y])
                S3.op("dve", lambda e: e.tensor_tensor(out=x1.t[:, half * 512:(half + 1) * 512], in0=Y.t[:], in1=xt.t[:, half * 512:(half + 1) * 512], op=ALU.add),
                      reads=[Y, xt], writes=[x1])
            for half in range(2):
                yhalf(half)
            S3.dma("sp", x1_d[r0:r0 + 128, :], x1.t[:], reads=[x1])
            S3.op("act", lambda e: e.activation(out=junk.t[:], in_=x1.t[:], func=AF.Square, accum_out=sst.t[:, blk:blk + 1]), reads=[x1], writes=[junk, sst])
            S3.op("act", lambda e: e.activation(out=sst.t[:, blk:blk + 1], in_=sst.t[:, blk:blk + 1], func=AF.Ln, scale=1.0 / 1024, bias=epsb.t[:, 0:1]), reads=[sst, epsb], writes=[sst])
            S3.op("act", lambda e: e.activation(out=sst.t[:, blk:blk + 1], in_=sst.t[:, blk:blk + 1], func=AF.Exp, scale=-0.5), reads=[sst], writes=[sst])
            h2 = h2pool.next()
            S3.op("act", lambda e: e.activation(out=h2.t[:], in_=x1.t[:], func=AF.Copy, scale=sst.t[:, blk:blk + 1]), reads=[x1, sst], writes=[h2])
            return dict(h2=h2, r0=r0, blk=blk)

        def p3_transposes(ctx, h2b):
            h2, blk = ctx["h2"], ctx["blk"]
            hT32 = h2T32p.next()
            ctx["hT32"] = hT32

            def thalf(half):
                pt = PTf.next()
                for c in range(4):
                    cc = half * 4 + c
                    S3.op("pe", lambda e, c=c, cc=cc: e.transpose(pt.t[:, c * 128:(c + 1) * 128], h2.t[:, cc * 128:(cc + 1) * 128], cstf.t[:]),
                          reads=[h2, cstf], writes=[pt])
                for c in range(4):
                    cc = half * 4 + c
                    S3.op("act", lambda e, c=c, cc=cc: e.activation(out=hT32.t[:, cc, :], in_=pt.t[:, c * 128:(c + 1) * 128], func=AF.Copy,
                                                                   scale=vecs3.t[:, V_GFFN + cc:V_GFFN + cc + 1]), reads=[pt, vecs3], writes=[hT32])
            for half in range(2):
                thalf(half)
            S3.op("pool", lambda e: e.tensor_copy(out=h2b.t[:, :, blk * 128:(blk + 1) * 128], in_=hT32.t[:]), reads=[hT32], writes=[h2b])

        def p3_router(ctx):
            hT32 = ctx["hT32"]
            PL = PLp.next()
            for k in range(8):
                S3.op("pe", lambda e, k=k: e.matmul(PL.t[:, 0:36], hT32.t[:, k, :], wr32.t[:, k, :], start=(k == 0), stop=(k == 7)),
                      reads=[hT32, wr32], writes=[PL])
            return p3_route(ctx["blk"], ctx["r0"], PL)

        def p3_loads(it):
            c0 = it * 512
            on = onp.next()
            for n, src in enumerate((osbT_d, olruT_d, oxT_d)):
                for c in range(4):
                    S3.dma("sp", on.t[:, n * 4 + c, :], src[c * 128:(c + 1) * 128, c0:c0 + 512], writes=[on], par=True)
            gts = []
            for j in range(8):
                gt = gtp.next()
                for n in range(3):
                    S3.dma("sp", gt.t[:, n, :], gT_d[(n * 8 + j) * 128:(n * 8 + j + 1) * 128, c0:c0 + 512], writes=[gt], par=True)
                gts.append(gt)
            return on, gts, mTp.next()

        cur = p3_loads(0)
        for j in range(8):
            p3_merge(j, cur[0], cur[2], cur[1][j])
        q1 = None
        q2 = None
        rgens = []

        def rstep():
            while rgens:
                try:
                    next(rgens[0])
                    return
                except StopIteration:
                    rgens.pop(0)

        def stage2(ctx):
            p3_transposes(ctx, ctx["h2b"])
            if ctx["blk"] == 3:
                c0_ = ctx["it"] * 512
                for c in range(8):
                    S3.dma("sp", h2T_d[c * 128:(c + 1) * 128, c0_:c0_ + 512], ctx["h2b"].t[:, c, :], reads=[ctx["h2b"]])

        allb = [(it, blk) for it in range(NT) for blk in range(4)]
        xts = {allb[0]: p3_xload(*allb[0])}
        for it in range(NT):
            nxt = p3_loads(it + 1) if it + 1 < NT else None
            sst = sspool.next()
            h2b = h2Tb.next()
            for blk in range(4):
                bi = it * 4 + blk
                if bi + 1 < len(allb):
                    xts[allb[bi + 1]] = p3_xload(*allb[bi + 1])
                ctx = p3_outproj(it, blk, cur[2], sst, xts.pop((it, blk)))
                ctx["h2b"] = h2b
                ctx["it"] = it
                rstep()
                if nxt is not None:
                    p3_merge(2 * blk, nxt[0], nxt[2], nxt[1][2 * blk])
                rstep()
                if q1 is not None:
                    stage2(q1)
                if nxt is not None:
                    p3_merge(2 * blk + 1, nxt[0], nxt[2], nxt[1][2 * blk + 1])
                rstep()
                if q2 is not None:
                    rgens.append(p3_router(q2))
                    rstep()
                q2 = q1
                q1 = ctx
            cur = nxt
        stage2(q1)
        if q2 is not None:
            rgens.append(p3_router(q2))
        rgens.append(p3_router(q1))
        while rgens:
            rstep()
        S3.finalize()

    if 4 in PH:
      ST = min(S, 2048)
      NST = S // ST
      NTT = ST // 256
      with ExitStack() as st:
        S4 = Sched(nc, sync_same_engine=SYNC_SAME)
        sb, ps = mk(st)
        h2T = sb("h2T4", [128, 8, ST], BF16)
        acc = sb("acc4", [128, ST // 128, 1024])
        accb = [Buf(f"acc{g}") for g in range(ST // 128)]
        cmb = sb("cmb4", [128, ST // 128, 32])
        sg_stage = sb("sgs", [128, 8, 256])
        su_stage = sb("sus", [128, 8, 256])
        sd_stage = sb("sds", [128, 2, 1024])
        wgp = Rot([sb(f"wg{i}", [128, 8, 256], BF16) for i in range(2)])
        wup = Rot([sb(f"wu{i}", [128, 8, 256], BF16) for i in range(2)])
        wdp = Rot([sb(f"wd{i}", [128, 2, 1024], BF16) for i in range(2)])
        sgp = Rot([sb(f"sg{i}", [128, 512]) for i in range(2)])
        actp = Rot([sb(f"at{i}", [128, 512], BF16) for i in range(3)])
        x1p = Rot([sb(f"x14_{i}", [128, 1024]) for i in range(4)])
        PG = Rot([ps(f"PG{i}") for i in range(2)])
        PU = Rot([ps(f"PU{i}") for i in range(2)])
        PD = Rot([ps(f"PD{i}") for i in range(4)])
        W = {}

        def load_w(ex):
            for c in range(8):
                S4.dma("sp", sg_stage.t[:, c, :], wg_d[ex, c * 128:(c + 1) * 128, :], writes=[sg_stage], par=True)
            for c in range(8):
                S4.dma("sp", su_stage.t[:, c, :], wu_d[ex, c * 128:(c + 1) * 128, :], writes=[su_stage], par=True)
            for c in range(2):
                S4.dma("sp", sd_stage.t[:, c, :], wd_d[ex, c * 128:(c + 1) * 128, :], writes=[sd_stage], par=True)

        def cast_w(key, which):
            pool_, stage = {"g": (wgp, sg_stage), "u": (wup, su_stage), "d": (wdp, sd_stage)}[which]
            wt = pool_.next()
            S4.op("act", lambda e: e.activation(out=wt.t[:], in_=stage.t[:], func=AF.Copy), reads=[stage], writes=[wt])
            W.setdefault(key, {})[which] = wt

        def gateup(t):
            ex, tc0 = t["ex"], t["j"] * 256
            wg, wu = W[t["key"]]["g"], W[t["key"]]["u"]
            Pg = PG.next()
            Pu = PU.next()
            for (Pq, wq) in ((Pg, wg), (Pu, wu)):
                for c2 in range(2):
                    for k in range(8):
                        S4.op("pe", lambda e, Pq=Pq, wq=wq, c2=c2, k=k: e.matmul(Pq.t[:, c2 * 256:(c2 + 1) * 256], wq.t[:, k, c2 * 128:(c2 + 1) * 128], h2T.t[:, k, tc0:tc0 + 256],
                                                                                 start=(k == 0), stop=(k == 7)), reads=[wq, h2T], writes=[Pq])
            sg = sgp.next()
            S4.op("act", lambda e: e.activation(out=sg.t[:], in_=Pg.t[:], func=AF.Silu), reads=[Pg], writes=[sg])
            at = actp.next()
            S4.op("dve", lambda e: e.tensor_tensor(out=at.t[:], in0=Pu.t[:], in1=sg.t[:], op=ALU.mult), reads=[Pu, sg], writes=[at])
            t["at"] = at

        def down(t):
            ex, tc0, at = t["ex"], t["j"] * 256, t["at"]
            wd = W[t["key"]]["d"]

            def one(blk, half):
                gb = (tc0 // 128) + blk
                Pd = PD.next()
                for c2 in range(2):
                    S4.op("pe", lambda e, c2=c2: e.matmul(Pd.t[:], at.t[:, c2 * 256 + blk * 128:c2 * 256 + (blk + 1) * 128],
                                                          wd.t[:, c2, half * 512:(half + 1) * 512], start=(c2 == 0), stop=(c2 == 1)),
                          reads=[at, wd], writes=[Pd])
                S4.op("dve", lambda e: e.scalar_tensor_tensor(out=acc.t[:, gb, half * 512:(half + 1) * 512], in0=Pd.t[:],
                                                             scalar=cmb.t[:, gb, ex:ex + 1], in1=acc.t[:, gb, half * 512:(half + 1) * 512],
                                                             op0=ALU.mult, op1=ALU.add), reads=[Pd, cmb, accb[gb]], writes=[accb[gb]])
            for blk in range(2):
                for half in range(2):
                    one(blk, half)

        X1 = {}

        def p4_x1load(t0, gb):
            r0 = t0 + gb * 128
            x1 = x1p.next()
            S4.dma("sp", x1.t[:], x1_d[r0:r0 + 128, :], writes=[x1])
            X1[(t0, gb)] = x1

        def p4_out(t0, gb):
            r0 = t0 + gb * 128
            if (t0, gb) not in X1:
                p4_x1load(t0, gb)
            x1 = X1.pop((t0, gb))
            S4.op("pool", lambda e: e.tensor_tensor(out=x1.t[:], in0=x1.t[:], in1=acc.t[:, gb, :], op=ALU.add), reads=[x1, accb[gb]], writes=[x1])
            S4.dma("sp", out_d[r0:r0 + 128, :], x1.t[:], reads=[x1])

        wsets = [(sti, ex) for sti in range(NST) for ex in range(n_exp)]
        load_w(wsets[0][1])
        for which in "gud":
            cast_w(wsets[0], which)
        for wi, (sti, ex) in enumerate(wsets):
            t0 = sti * ST
            if ex == 0:
                for c in range(8):
                    S4.dma("sp", h2T.t[:, c, :], h2T_d[c * 128:(c + 1) * 128, t0:t0 + ST], writes=[h2T], par=True)
                for gb in range(ST // 128):
                    S4.dma("sp", cmb.t[:, gb, :], comb_d[t0 + gb * 128:t0 + (gb + 1) * 128, :], writes=[cmb], par=True)
                for gb in range(ST // 128):
                    S4.op("dve", lambda e, gb=gb: e.memset(acc.t[:, gb, :], 0.0), writes=[accb[gb]])
            nxt = wsets[wi + 1] if wi + 1 < len(wsets) else None
            tiles = [dict(ex=ex, j=j, key=(sti, ex)) for j in range(NTT)]
            if ex == n_exp - 1:
                for gb in range(min(4, ST // 128)):
                    p4_x1load(t0, gb)
            prev = None
            for j, t in enumerate(tiles):
                if nxt is not None:
                    if j == 0:
                        load_w(nxt[1])
                    if j == 1 % NTT:
                        cast_w(nxt, "g")
                    if j == 2 % NTT:
                        cast_w(nxt, "u")
                    if j == 3 % NTT:
                        cast_w(nxt, "d")
                gateup(t)
                if prev is not None:
                    down(prev)
                prev = t
            down(prev)
            if ex == n_exp - 1:
                for gb in range(ST // 128):
                    p4_out(t0, gb)
        S4.finalize()
    return nc


def _host_consts():
    j = np.arange(128)[:, None]
    s = np.arange(128)[None, :]
    c = np.zeros((128, 8, 128), np.float32)
    c[:, 0, :] = np.eye(128, dtype=np.float32)
    c[:, 1, :] = np.where(j >= s, -1.0, 0.0)
    c[:, 2, :] = -1.0
    c[:, 3, :] = np.where(j < s, 1.0, 0.0)
    c[:, 4, :] = np.where((j // 64) == (s // 64), 1.0, 0.0)
    c[:, 5, :] = 1.0
    c[:, 6, :] = c[:, 3, :]
    c[:, 7, :] = c[:, 3, :]
    return c


def _shared_inputs(inp):
    f = np.float32
    vecs = np.zeros((128, NVEC), f)
    vecs[:, 0] = np.tile(inp["g_q_sb"][0], 2)
    vecs[:, 1] = np.tile(inp["g_k_sb"][0], 2)
    vecs[:, 2] = inp["g_q_x"][0]
    vecs[:, 3] = inp["g_k_x"][0]
    cw = inp["conv_w"][0]
    for c in range(4):
        for k in range(4):
            vecs[:, 4 + c * 4 + k] = cw[k, c * 128:(c + 1) * 128]
    vecs[:, 20:24] = inp["conv_b"][0].reshape(4, 128).T
    vecs[:, 24:28] = inp["lru_b_a"][0].reshape(4, 128).T
    vecs[:, 28:32] = inp["lru_b_i"][0].reshape(4, 128).T
    vecs[:, 32:36] = inp["lru_lambda"][0].reshape(4, 128).T
    vecs[:, 36:44] = inp["g_ffn"][0].reshape(8, 128).T
    gfull = np.zeros((128, 2, 1024), f)
    gfull[:, 0, :] = inp["g_mix"][0][None, :]
    gfull[:, 1, :] = inp["g_ffn"][0][None, :]
    gmemfull = np.ascontiguousarray(np.broadcast_to(inp["g_mem"][0][None, :], (128, 1024))).astype(f)
    rbias = np.ascontiguousarray(np.broadcast_to(np.concatenate([inp["b_group"][0], inp["b_expert"][0]])[None, :], (128, 36))).astype(f)
    wabd = np.zeros((128, 8, 128), f)
    for t, w in enumerate((inp["lru_w_a"][0], inp["lru_w_i"][0])):
        for c in range(4):
            for bb in range(2):
                wabd[bb * 64:(bb + 1) * 64, t * 4 + c, bb * 64:(bb + 1) * 64] = w[2 * c + bb]
    return {
        "w_in": np.ascontiguousarray(inp["w_in"][0]),
        "vecs": vecs, "gfull": gfull, "gmemfull": gmemfull, "rbias": rbias, "wabd": wabd,
        "w_mem_kv": np.ascontiguousarray(inp["w_mem_kv"][0]),
        "w_branch": np.ascontiguousarray(inp["w_branch"][0]),
        "w_out": np.ascontiguousarray(inp["w_out"][0]),
        "w_router": np.ascontiguousarray(np.concatenate([inp["w_group"][0], inp["w_expert"][0]], axis=1)),
        "w_gate": np.ascontiguousarray(inp["w_gate"][0].reshape(32, 1024, 256)),
        "w_up": np.ascontiguousarray(inp["w_up"][0].reshape(32, 1024, 256)),
        "w_down": np.ascontiguousarray(inp["w_down"][0].reshape(32, 256, 1024)),
        "consts": _host_consts(),
    }


def kernel(**inputs):
    inp = {k: np.asarray(v, dtype=np.float32) for k, v in inputs.items()}
    B, S, D = inp["x"].shape
    shared = _shared_inputs(inp)
    nc = build(S)
    in_maps = []
    for b in range(B):
        m = dict(shared)
        m["x"] = np.ascontiguousarray(inp["x"][b])
        m["mem"] = np.ascontiguousarray(inp["mem"][b])
        in_maps.append(m)
    res = run_bass_kernel_spmd(nc, in_maps, core_ids=list(range(B)))
    return np.stack([np.asarray(r["out"], dtype=np.float32) for r in res.results], axis=0)
```

```python
import contextlib
from contextlib import ExitStack
import numpy as np
import concourse.bass as bass
import concourse.mybir as mybir
from concourse.bass_utils import run_bass_kernel_spmd

F32 = mybir.dt.float32
BF16 = mybir.dt.bfloat16
AF = mybir.ActivationFunctionType
ALU = mybir.AluOpType
AX = mybir.AxisListType

SEM_CH = 30000
SYNC_SAME = True
N_DMA_SEMS = 6
EPS = 1e-6
BIG = 1.0e9


class Buf:
    __slots__ = ("name", "writes", "reads")

    def __init__(self, name=""):
        self.name = name
        self.writes = []
        self.reads = []


class TT:
    __slots__ = ("t", "b")

    def __init__(self, t, name=""):
        self.t = t
        self.b = Buf(name)


class Rot:
    def __init__(self, items):
        self.items = items
        self.i = 0

    def next(self):
        x = self.items[self.i % len(self.items)]
        self.i += 1
        return x


class SemPool:
    def __init__(self, nc, n):
        self.h = [nc.alloc_semaphore(name=f"gsem{i}") for i in range(n)]
        self.i = 0

    def take(self):
        h = self.h[self.i]
        self.i += 1
        return h

    def reset(self, nc):
        hs = self.h[:self.i]
        with nc.Block() as block:
            def body(g):
                for h in hs:
                    g.sem_clear(h)
            block.gpsimd(body)
        self.i = 0


class Sched:
    ENGS = ("pe", "act", "dve", "pool", "sp")

    def __init__(self, nc, sync_same_engine=True):
        self.nc = nc
        self.sempool = nc._sempool
        self.streams = {e: [] for e in self.ENGS}
        self.count = {e: 0 for e in self.ENGS}
        self.dma_count = {}
        self.sync_same = sync_same_engine
        self.dma_rr = {e: 0 for e in self.ENGS}
        self.waited = {}

    def _deps_for(self, reads, writes, par=False):
        deps = []
        for b in reads:
            deps.extend(b.writes)
        for b in writes:
            if not par:
                deps.extend(b.writes)
            deps.extend(b.reads)
        best = {}
        for d in deps:
            k = (d[0], d[1])
            if k not in best or best[k][2] < d[2]:
                best[k] = d
        return list(best.values())

    def _emit(self, eng, fn, deps, is_dma, dma_q=None):
        waits = []
        for d in deps:
            if d is None:
                continue
            if d[0] == "E":
                _, e2, n = d
                if e2 == eng and (eng == "pe" or (not self.sync_same and eng != "pool")) and not is_dma:
                    continue
                key = (eng, "E", e2)
            else:
                key = (eng, "D", d[1])
                n = d[2]
            if self.waited.get(key, 0) >= n:
                continue
            self.waited[key] = n
            waits.append(d)
        if is_dma:
            k = dma_q
            self.dma_count[k] = self.dma_count.get(k, 0) + 1
            tok = ("D", k, self.dma_count[k] * 16)
        else:
            self.count[eng] += 1
            tok = ("E", eng, self.count[eng])
        self.streams[eng].append((fn, waits, tok))
        return tok

    def _post(self, tok, reads, writes, par=False):
        for b in reads:
            b.reads.append(tok)
        for b in writes:
            if par and not b.reads:
                b.writes.append(tok)
            else:
                b.writes = [tok]
            b.reads = []

    def op(self, eng, fn, reads=(), writes=()):
        reads = [r.b if isinstance(r, TT) else r for r in reads]
        writes = [w.b if isinstance(w, TT) else w for w in writes]
        tok = self._emit(eng, fn, self._deps_for(reads, writes), False)
        self._post(tok, reads, writes)
        return tok

    def dma(self, eng, out, in_, reads=(), writes=(), par=False):
        reads = [r.b if isinstance(r, TT) else r for r in reads]
        writes = [w.b if isinstance(w, TT) else w for w in writes]
        deps = self._deps_for(reads, writes, par)
        base = {"sp": 0, "pool": N_DMA_SEMS, "act": 2 * N_DMA_SEMS}[eng]
        k = base + self.dma_rr[eng]
        self.dma_rr[eng] = (self.dma_rr[eng] + 1) % N_DMA_SEMS
        prev = self.dma_count.get(k, 0)
        if prev > 0:
            deps.append(("D", k, prev * 16))
        tok = self._emit(eng, lambda e, o=out, i=in_: e.dma_start(out=o, in_=i), deps, True, dma_q=k)
        self._post(tok, reads, writes, par)
        return tok

    def finalize(self, final_eng="sp"):
        nc = self.nc
        fin_waits = [("D", k, v * 16) for k, v in self.dma_count.items()]
        with ExitStack() as st:
            esems = {}
            for e in self.ENGS:
                n = (self.count[e] // SEM_CH) + 1
                esems[e] = [self.sempool.take() for i in range(n)]
            dsems = {k: self.sempool.take() for k in sorted(self.dma_count)}
            block = st.enter_context(nc.Block())

            def sem_for(tok):
                if tok[0] == "E":
                    _, e2, n = tok
                    idx = (n - 1) // SEM_CH
                    return esems[e2][idx], n - idx * SEM_CH
                return dsems[tok[1]], tok[2]

            def run(engname):
                def body(eng):
                    for fn, waits, tok in self.streams[engname]:
                        for w in waits:
                            s, v = sem_for(w)
                            eng.wait_ge(s, v)
                        ins = fn(eng)
                        s, v = sem_for(tok)
                        ins.then_inc(s, 1 if tok[0] == "E" else 16)
                    if engname == final_eng:
                        for w in fin_waits:
                            s, v = sem_for(w)
                            eng.wait_ge(s, v)
                return body

            block.tensor(run("pe"))
            block.scalar(run("act"))
            block.vector(run("dve"))
            block.gpsimd(run("pool"))
            block.sync(run("sp"))


NVEC = 8 * 3 + 4 + 16 + 4 * 4


def build(S, debug=False, phases=(0, 1, 2, 3, 4), n_exp=32):
    assert S % 512 == 0
    NT = S // 512
    NB = S // 128
    nc = bass.Bass("TRN2", target_bir_lowering=False)
    nc._sempool = SemPool(nc, 96)

    def din(name, shape):
        return nc.dram_tensor(name, shape, F32, kind="ExternalInput").ap()

    x_d = din("x", [S, 1024])
    mem_d = din("mem", [256, 1024])
    w_in_d = din("w_in", [1024, 6144])
    vecs_d = din("vecs", [128, NVEC])
    gfull_d = din("gfull", [128, 2, 1024])
    gmemfull_d = din("gmemfull", [128, 1024])
    rbias_d = din("rbias", [128, 36])
    wabd_d = din("wabd", [128, 8, 128])
    wkv_d = din("w_mem_kv", [1024, 1024])
    wbr_d = din("w_branch", [3, 512, 1024])
    wout_d = din("w_out", [1024, 1024])
    wr_d = din("w_router", [1024, 36])
    wg_d = din("w_gate", [32, 1024, 256])
    wu_d = din("w_up", [32, 1024, 256])
    wd_d = din("w_down", [32, 256, 1024])
    consts_d = din("consts", [128, 8, 128])
    out_d = nc.dram_tensor("out", [S, 1024], F32, kind="ExternalOutput").ap()

    skind = "ExternalOutput" if debug else "Internal"

    def dscr(name, shape, dt):
        return nc.dram_tensor(name, shape, dt, kind=skind).ap()

    qT_d = dscr("qT", [512, S], BF16)
    kT_d = dscr("kT", [512, S], BF16)
    v_d = dscr("v", [S, 512], BF16)
    olruT_d = dscr("olruT", [512, S], BF16)
    oxT_d = dscr("oxT", [512, S], BF16)
    gT_d = dscr("gT", [3072, S], BF16)
    osbT_d = dscr("osbT", [512, S], BF16)
    x1_d = dscr("x1", [S, 1024], F32)
    h2T_d = dscr("h2T", [1024, S], BF16)
    comb_d = dscr("comb", [S, 32], F32)

    V_GQSB, V_GKSB, V_GQX, V_GKX = 0, 1, 2, 3
    V_CONVW = 4
    V_CONVB = 20
    V_BA, V_BI, V_LAM = 24, 28, 32
    V_GFFN = 36

    PH = phases

    def mk(st):
        def sb(name, shape, dt=F32):
            return TT(st.enter_context(nc.sbuf_tensor("sb_" + name, shape, dt)), name)

        def ps(name, dt=F32, n=512):
            return TT(st.enter_context(nc.psum_tensor("ps_" + name, [128, n], dt)), name)
        return sb, ps

    def make_norm_helpers(SX, junk, sc, cst, wk16, wk32, PA):
        def rms_rstd(src_tt, src_ap, sstile, col, nfeat):
            SX.op("act", lambda e: e.activation(out=junk.t[:, 0:nfeat], in_=src_ap, func=AF.Square, accum_out=sstile.t[:, col:col + 1]),
                  reads=[src_tt], writes=[junk, sstile])
            SX.op("act", lambda e: e.activation(out=sstile.t[:, col:col + 1], in_=sstile.t[:, col:col + 1], func=AF.Ln, scale=1.0 / nfeat, bias=epsb.t[:, 0:1]),
                  reads=[sstile, epsb], writes=[sstile])
            SX.op("act", lambda e: e.activation(out=sstile.t[:, col:col + 1], in_=sstile.t[:, col:col + 1], func=AF.Exp, scale=-0.5),
                  reads=[sstile], writes=[sstile])

        def feat_norm_a(P, ncols):
            sq = wk16.next()
            SX.op("act", lambda e: e.activation(out=sq.t[:, 0:ncols], in_=P.t[:, 0:ncols], func=AF.Square), reads=[P], writes=[sq])
            return sq

        def feat_norm_b(P, sq, ncols, onesmat, nfeat, gcol, out_tt, out16):
            A = PA.next()
            SX.op("pe", lambda e: e.matmul(A.t[:, 0:ncols], onesmat, sq.t[:, 0:ncols], start=True, stop=True), reads=[cst, sq], writes=[A])
            ln = wk32.next()
            SX.op("act", lambda e: e.activation(out=ln.t[:, 0:ncols], in_=A.t[:, 0:ncols], func=AF.Ln, scale=1.0 / nfeat, bias=epsb.t[:, 0:1]), reads=[A, epsb], writes=[ln])
            r = wk32.next()
            SX.op("act", lambda e: e.activation(out=r.t[:, 0:ncols], in_=ln.t[:, 0:ncols], func=AF.Exp, scale=-0.5), reads=[ln], writes=[r])
            SX.op("dve", lambda e: e.scalar_tensor_tensor(out=out16, in0=P.t[:, 0:ncols], scalar=sc.t[:, gcol:gcol + 1], in1=r.t[:, 0:ncols],
                                                         op0=ALU.mult, op1=ALU.mult), reads=[P, sc, r], writes=[out_tt])

        def feat_norm(P, ncols, onesmat, nfeat, gcol, out_tt, out16):
            sq = feat_norm_a(P, ncols)
            feat_norm_b(P, sq, ncols, onesmat, nfeat, gcol, out_tt, out16)
        feat_norm.a = feat_norm_a
        feat_norm.b = feat_norm_b
        return rms_rstd, feat_norm

    with ExitStack() as stO:
        sbO, psO = mk(stO)
        vecs = sbO("vecs", [128, NVEC])
        cst = sbO("cst", [128, 8, 128], BF16)
        kmem = sbO("kmem", [128, 4, 256], BF16)
        vmem = sbO("vmem", [128, 2, 512], BF16)
        sc = sbO("sc", [128, 16])
        epsb = sbO("epsb", [128, 1])
        junk = sbO("junk", [128, 1024])
        ident = cst.t[:, 0, :]
        ones64 = cst.t[:, 4, :]
        ones128 = cst.t[:, 5, :]

        if 0 in PH:
          with ExitStack() as st:
            S0 = Sched(nc, sync_same_engine=SYNC_SAME)
            sb, ps = mk(st)
            gmemf = sb("gmemf", [128, 1024])
            wkv = sb("wkv", [128, 8, 1024], BF16)
            memT = sb("memT", [128, 8, 256], BF16)
            xpool = Rot([sb(f"xm{i}", [128, 1024]) for i in range(2)])
            hnpool = Rot([sb(f"hm{i}", [128, 1024], BF16) for i in range(2)])
            ssm = sb("ssm", [128, 4])
            wk32 = Rot([sb(f"wk0_{i}", [128, 512]) for i in range(4)])
            wk16 = Rot([sb(f"wb0_{i}", [128, 512], BF16) for i in range(2)])
            PT = Rot([ps(f"PT0_{i}", BF16, 1024) for i in range(2)])
            PM = Rot([ps(f"PM0_{i}") for i in range(4)])
            PA = Rot([ps(f"PA0_{i}") for i in range(2)])
            rms_rstd, feat_norm = make_norm_helpers(S0, junk, sc, cst, wk16, wk32, PA)

            S0.dma("sp", vecs.t[:], vecs_d, writes=[vecs])
            S0.dma("sp", gmemf.t[:], gmemfull_d, writes=[gmemf])
            S0.dma("pool", cst.t[:], consts_d, writes=[cst])
            for c in range(8):
                S0.dma("pool", wkv.t[:, c, :], wkv_d[c * 128:(c + 1) * 128, :], writes=[wkv], par=True)
            S0.op("pool", lambda e: e.memset(epsb.t[:], EPS), writes=[epsb])
            S0.op("dve", lambda e: e.tensor_scalar(out=sc.t[:, 0:1], in0=vecs.t[:, V_GQSB:V_GQSB + 1], scalar1=0.125, scalar2=None, op0=ALU.mult),
                  reads=[vecs], writes=[sc])
            S0.op("dve", lambda e: e.tensor_copy(out=sc.t[:, 1:2], in_=vecs.t[:, V_GKSB:V_GKSB + 1]), reads=[vecs], writes=[sc])
            S0.op("dve", lambda e: e.tensor_scalar(out=sc.t[:, 2:3], in0=vecs.t[:, V_GQX:V_GQX + 1], scalar1=float(128 ** -0.5), scalar2=None, op0=ALU.mult),
                  reads=[vecs], writes=[sc])
            S0.op("dve", lambda e: e.tensor_copy(out=sc.t[:, 3:4], in_=vecs.t[:, V_GKX:V_GKX + 1]), reads=[vecs], writes=[sc])
            S0.op("act", lambda e: e.activation(out=sc.t[:, 12:16], in_=vecs.t[:, V_LAM:V_LAM + 4], func=AF.Exp, scale=-1.0), reads=[vecs], writes=[sc])
            S0.op("act", lambda e: e.activation(out=sc.t[:, 12:16], in_=sc.t[:, 12:16], func=AF.Ln, bias=1.0), reads=[sc], writes=[sc])
            S0.op("dve", lambda e: e.tensor_scalar(out=sc.t[:, 4:8], in0=sc.t[:, 12:16], scalar1=-8.0, scalar2=None, op0=ALU.mult), reads=[sc], writes=[sc])
            S0.op("dve", lambda e: e.tensor_scalar(out=sc.t[:, 8:12], in0=sc.t[:, 12:16], scalar1=-16.0, scalar2=None, op0=ALU.mult), reads=[sc], writes=[sc])

            def p0_blk(blk):
                xt = xpool.next()
                S0.dma("sp", xt.t[:], mem_d[blk * 128:(blk + 1) * 128, :], writes=[xt])
                rms_rstd(xt, xt.t[:], ssm, blk, 1024)
                hn = hnpool.next()
                S0.op("dve", lambda e: e.scalar_tensor_tensor(out=hn.t[:], in0=xt.t[:], scalar=ssm.t[:, blk:blk + 1], in1=gmemf.t[:],
                                                             op0=ALU.mult, op1=ALU.mult), reads=[xt, ssm, gmemf], writes=[hn])
                pt = PT.next()
                for c in range(8):
                    S0.op("pe", lambda e, c=c: e.transpose(pt.t[:, c * 128:(c + 1) * 128], hn.t[:, c * 128:(c + 1) * 128], ident),
                          reads=[hn, cst], writes=[pt])
                S0.op("dve", lambda e: e.tensor_copy(out=memT.t[:, :, blk * 128:(blk + 1) * 128],
                                                     in_=pt.t[:].rearrange("p (c n) -> p c n", c=8)), reads=[pt], writes=[memT])
            for blk in range(2):
                p0_blk(blk)

            def p0_k(h):
                P = PM.next()
                for k in range(8):
                    S0.op("pe", lambda e, k=k: e.matmul(P.t[:, 0:256], wkv.t[:, k, h * 128:(h + 1) * 128], memT.t[:, k, :], start=(k == 0), stop=(k == 7)),
                          reads=[wkv, memT], writes=[P])
                feat_norm(P, 256, ones128, 128, 3, kmem, kmem.t[:, h, :])
            for h in range(4):
                p0_k(h)

            def p0_v(blk):
                P = PM.next()
                for k in range(8):
                    S0.op("pe", lambda e, k=k: e.matmul(P.t[:], memT.t[:, k, blk * 128:(blk + 1) * 128], wkv.t[:, k, 512:1024], start=(k == 0), stop=(k == 7)),
                          reads=[wkv, memT], writes=[P])
                S0.op("act", lambda e: e.activation(out=vmem.t[:, blk, :], in_=P.t[:], func=AF.Copy), reads=[P], writes=[vmem])
            for blk in range(2):
                p0_v(blk)
            S0.finalize()
            for t in (vecs, cst, kmem, vmem, sc, epsb, junk):
                t.b.writes = []
                t.b.reads = []

        if 1 in PH:
          with ExitStack() as st:
            S1 = Sched(nc, sync_same_engine=SYNC_SAME)
            sb, ps = mk(st)
            win = sb("win", [128, 8, 6144], BF16)
            gmixf = sb("gmixf", [128, 1024])
            wabd = sb("wabd", [128, 8, 128], BF16)
            xbuf = [sb(f"xbuf{c}", [128, 515]) for c in range(4)]
            state = sb("state", [128, 4])
            hT = Rot([sb(f"hT{i}", [128, 8, 512], BF16) for i in range(2)])
            xpool = Rot([sb(f"xt{i}", [128, 1024]) for i in range(3)])
            hnpool = Rot([sb(f"hn{i}", [128, 1024], BF16) for i in range(2)])
            sspool = Rot([sb(f"ss{i}", [128, 4]) for i in range(3)])
            wk32 = Rot([sb(f"wk{i}", [128, 512]) for i in range(10)])
            wk16 = Rot([sb(f"wb{i}", [128, 512], BF16) for i in range(6)])
            ob16 = Rot([sb(f"ob{i}", [128, 512], BF16) for i in range(6)])
            PT = Rot([ps(f"PT{i}", BF16, 1024) for i in range(1)])
            PM = Rot([ps(f"PM{i}") for i in range(5)])
            PA = Rot([ps(f"PA{i}") for i in range(2)])
            rms_rstd, feat_norm = make_norm_helpers(S1, junk, sc, cst, wk16, wk32, PA)

            S1.dma("sp", gmixf.t[:], gfull_d[:, 0, :], writes=[gmixf])
            S1.dma("pool", wabd.t[:], wabd_d, writes=[wabd])
            for c in range(8):
                for hf in range(2):
                    S1.dma("pool", win.t[:, c, hf * 3072:(hf + 1) * 3072], w_in_d[c * 128:(c + 1) * 128, hf * 3072:(hf + 1) * 3072], writes=[win], par=True)
            for c in range(4):
                S1.op("pool", lambda e, c=c: e.memset(xbuf[c].t[:, 0:3], 0.0), writes=[xbuf[c]])
            S1.op("pool", lambda e: e.memset(state.t[:], 0.0), writes=[state])

            def p1_xblk(it, blk, hTt, sst):
                r0 = it * 512 + blk * 128
                xt = xpool.next()
                S1.dma("sp", xt.t[:], x_d[r0:r0 + 128, :], writes=[xt])
                rms_rstd(xt, xt.t[:], sst, blk, 1024)
                hn = hnpool.next()
                S1.op("dve", lambda e: e.scalar_tensor_tensor(out=hn.t[:], in0=xt.t[:], scalar=sst.t[:, blk:blk + 1], in1=gmixf.t[:],
                                                             op0=ALU.mult, op1=ALU.mult), reads=[xt, sst, gmixf], writes=[hn])
                pt = PT.next()
                for c in range(8):
                    S1.op("pe", lambda e, c=c: e.transpose(pt.t[:, c * 128:(c + 1) * 128], hn.t[:, c * 128:(c + 1) * 128], ident),
                          reads=[hn, cst], writes=[pt])
                S1.op("dve", lambda e: e.tensor_copy(out=hTt.t[:, :, blk * 128:(blk + 1) * 128],
                                                     in_=pt.t[:].rearrange("p (c n) -> p c n", c=8)), reads=[pt], writes=[hTt])

            def proj_fm(ci, hTt):
                P = PM.next()
                for k in range(8):
                    S1.op("pe", lambda e, k=k: e.matmul(P.t[:], win.t[:, k, ci * 128:(ci + 1) * 128], hTt.t[:, k, :], start=(k == 0), stop=(k == 7)),
                          reads=[win, hTt], writes=[P])
                return P

            def p1_qk(ci, hTt, c0):
                L = {}

                def a():
                    L["P"] = proj_fm(ci, hTt)
                    L["sq"] = feat_norm.a(L["P"], 512)

                def b():
                    o = ob16.next()
                    feat_norm.b(L["P"], L["sq"], 512, ones64, 64, 0 if ci < 4 else 1, o, o.t[:])
                    dst = (qT_d if ci < 4 else kT_d)[(ci % 4) * 128:(ci % 4 + 1) * 128, c0:c0 + 512]
                    S1.dma("sp", dst, o.t[:], reads=[o])
                return [a, b]

            def p1_v(blk, hTt, c0):
                P = PM.next()
                for k in range(8):
                    S1.op("pe", lambda e, k=k: e.matmul(P.t[:], hTt.t[:, k, blk * 128:(blk + 1) * 128], win.t[:, k, 1024:1536], start=(k == 0), stop=(k == 7)),
                          reads=[win, hTt], writes=[P])
                o = ob16.next()
                S1.op("act", lambda e: e.activation(out=o.t[:], in_=P.t[:], func=AF.Copy), reads=[P], writes=[o])
                S1.dma("sp", v_d[c0 + blk * 128:c0 + (blk + 1) * 128, :], o.t[:], reads=[o])

            def p1_qx(h, hTt, c0):
                L = {}

                def a():
                    L["P"] = proj_fm(20 + h, hTt)
                    L["sq"] = feat_norm.a(L["P"], 512)

                def a2():
                    qn = wk16.next()
                    L["qn"] = qn
                    feat_norm.b(L["P"], L["sq"], 512, ones128, 128, 2, qn, qn.t[:])

                def b():
                    qn = L["qn"]
                    pms = []

                    def sx(mb):
                        Sx = PM.next()
                        S1.op("pe", lambda e: e.matmul(Sx.t[:], kmem.t[:, h, mb * 128:(mb + 1) * 128], qn.t[:], start=True, stop=True),
                              reads=[kmem, qn], writes=[Sx])
                        pm = wk16.next()
                        S1.op("act", lambda e: e.activation(out=pm.t[:], in_=Sx.t[:], func=AF.Exp), reads=[Sx], writes=[pm])
                        pms.append(pm)
                    for mb in range(2):
                        sx(mb)
                    L["pms"] = pms

                def c():
                    pms = L["pms"]
                    Ox = PM.next()
                    Dn = PA.next()
                    for mb in range(2):
                        S1.op("pe", lambda e, mb=mb: e.matmul(Ox.t[:], vmem.t[:, mb, h * 128:(h + 1) * 128], pms[mb].t[:], start=(mb == 0), stop=(mb == 1)),
                              reads=[vmem, pms[mb]], writes=[Ox])
                    for mb in range(2):
                        S1.op("pe", lambda e, mb=mb: e.matmul(Dn.t[:], ones128, pms[mb].t[:], start=(mb == 0), stop=(mb == 1)),
                              reads=[cst, pms[mb]], writes=[Dn])
                    rd = wk32.next()
                    S1.op("dve", lambda e: e.reciprocal(out=rd.t[:], in_=Dn.t[:]), reads=[Dn], writes=[rd])
                    o = ob16.next()
                    S1.op("dve", lambda e: e.tensor_tensor(out=o.t[:], in0=Ox.t[:], in1=rd.t[:], op=ALU.mult), reads=[Ox, rd], writes=[o])
                    S1.dma("sp", oxT_d[h * 128:(h + 1) * 128, c0:c0 + 512], o.t[:], reads=[o])
                return [a, a2, b, c]

            def p1_gate(j, hTt, c0):
                P = proj_fm(24 + j, hTt)
                o = ob16.next()
                S1.op("act", lambda e: e.activation(out=o.t[:], in_=P.t[:], func=AF.Sigmoid), reads=[P], writes=[o])
                S1.dma("sp", gT_d[j * 128:(j + 1) * 128, c0:c0 + 512], o.t[:], reads=[o])

            def lru_stages(c, hTt, c0):
                L = {}
                xb = xbuf[c]
                cw = V_CONVW + c * 4

                def s0():
                    Px = proj_fm(12 + c, hTt)
                    S1.op("act", lambda e: e.activation(out=xb.t[:, 3:515], in_=Px.t[:], func=AF.Copy), reads=[Px], writes=[xb])
                    Py = proj_fm(16 + c, hTt)
                    gy = wk32.next()
                    S1.op("act", lambda e: e.activation(out=gy.t[:], in_=Py.t[:], func=AF.Gelu_apprx_tanh), reads=[Py], writes=[gy])
                    L["gy"] = gy

                def s1():
                    xc = wk32.next()
                    L["xc"] = xc
                    S1.op("dve", lambda e: e.tensor_scalar(out=xc.t[:], in0=xb.t[:, 0:512], scalar1=vecs.t[:, cw:cw + 1],
                                                          scalar2=vecs.t[:, V_CONVB + c:V_CONVB + c + 1], op0=ALU.mult, op1=ALU.add),
                          reads=[xb, vecs], writes=[xc])
                    for k in range(1, 4):
                        S1.op("dve", lambda e, k=k: e.scalar_tensor_tensor(out=xc.t[:], in0=xb.t[:, k:k + 512], scalar=vecs.t[:, cw + k:cw + k + 1],
                                                                          in1=xc.t[:], op0=ALU.mult, op1=ALU.add),
                              reads=[xb, vecs, xc], writes=[xc])
                    S1.op("dve", lambda e: e.tensor_copy(out=xb.t[:, 0:3], in_=xb.t[:, 512:515]), reads=[xb], writes=[xb])
                    xcb = wk16.next()
                    L["xcb"] = xcb
                    S1.op("act", lambda e: e.activation(out=xcb.t[:], in_=xc.t[:], func=AF.Copy), reads=[xc], writes=[xcb])

                def s2():
                    xcb = L["xcb"]
                    Pr = PA.next()
                    S1.op("pe", lambda e: e.matmul(Pr.t[:], wabd.t[:, c, :], xcb.t[:], start=True, stop=True), reads=[wabd, xcb], writes=[Pr])
                    Pi = PA.next()
                    S1.op("pe", lambda e: e.matmul(Pi.t[:], wabd.t[:, 4 + c, :], xcb.t[:], start=True, stop=True), reads=[wabd, xcb], writes=[Pi])
                    rr = wk32.next()
                    S1.op("act", lambda e: e.activation(out=rr.t[:], in_=Pr.t[:], func=AF.Sigmoid, bias=vecs.t[:, V_BA + c:V_BA + c + 1]),
                          reads=[Pr, vecs], writes=[rr])
                    ii = wk32.next()
                    S1.op("act", lambda e: e.activation(out=ii.t[:], in_=Pi.t[:], func=AF.Sigmoid, bias=vecs.t[:, V_BI + c:V_BI + c + 1]),
                          reads=[Pi, vecs], writes=[ii])
                    L["rr"], L["ii"] = rr, ii

                def s3():
                    rr = L["rr"]
                    aa = wk32.next()
                    S1.op("act", lambda e: e.activation(out=aa.t[:], in_=rr.t[:], func=AF.Exp, scale=sc.t[:, 4 + c:5 + c]), reads=[rr, sc], writes=[aa])
                    a2 = wk32.next()
                    S1.op("act", lambda e: e.activation(out=a2.t[:], in_=rr.t[:], func=AF.Exp, scale=sc.t[:, 8 + c:9 + c]), reads=[rr, sc], writes=[a2])
                    S1.op("act", lambda e: e.activation(out=a2.t[:], in_=a2.t[:], func=AF.Ln, scale=-1.0, bias=1.0), reads=[a2], writes=[a2])
                    S1.op("act", lambda e: e.activation(out=a2.t[:], in_=a2.t[:], func=AF.Exp, scale=0.5), reads=[a2], writes=[a2])
                    L["aa"], L["a2"] = aa, a2

                def s4():
                    ii, xc, a2, aa = L["ii"], L["xc"], L["a2"], L["aa"]
                    S1.op("dve", lambda e: e.tensor_tensor(out=ii.t[:], in0=ii.t[:], in1=xc.t[:], op=ALU.mult), reads=[ii, xc], writes=[ii])
                    S1.op("dve", lambda e: e.tensor_tensor(out=ii.t[:], in0=ii.t[:], in1=a2.t[:], op=ALU.mult), reads=[ii, a2], writes=[ii])
                    hs = wk32.next()
                    L["hs"] = hs
                    S1.op("dve", lambda e: e.tensor_tensor_scan(out=hs.t[:], data0=aa.t[:], data1=ii.t[:], initial=state.t[:, c:c + 1],
                                                               op0=ALU.mult, op1=ALU.add), reads=[aa, ii, state], writes=[hs])
                    S1.op("dve", lambda e: e.tensor_copy(out=state.t[:, c:c + 1], in_=hs.t[:, 511:512]), reads=[hs], writes=[state])

                def s5():
                    hs, gy = L["hs"], L["gy"]
                    o = ob16.next()
                    S1.op("dve", lambda e: e.tensor_tensor(out=o.t[:], in0=hs.t[:], in1=gy.t[:], op=ALU.mult), reads=[hs, gy], writes=[o])
                    S1.dma("sp", olruT_d[c * 128:(c + 1) * 128, c0:c0 + 512], o.t[:], reads=[o])
                return [s0, s1, s2, s3, s4, s5]

            def p1_front(it):
                hTt = hT.next()
                sst = sspool.next()
                for blk in range(4):
                    p1_xblk(it, blk, hTt, sst)
                return hTt

            hT_next = p1_front(0)
            for it in range(NT):
                c0 = it * 512
                hTt = hT_next
                items = []
                for i4 in range(4):
                    items.append(p1_qx(i4, hTt, c0))
                    items.append(p1_qk(2 * i4, hTt, c0))
                    items.append(p1_qk(2 * i4 + 1, hTt, c0))
                    items.append([lambda blk=i4: p1_v(blk, hTt, c0)])
                active = []
                while items or active:
                    while items and len(active) < 3:
                        active.append(items.pop(0))
                    for itm in list(active):
                        itm.pop(0)()
                        if not itm:
                            active.remove(itm)
                stages = []
                for j in range(24):
                    if j % 6 == 0:
                        stages = lru_stages(j // 6, hTt, c0)
                    p1_gate(j, hTt, c0)
                    stages[j % 6]()
                    if j == 9 and it + 1 < NT:
                        hT_next = p1_front(it + 1)
            S1.finalize()

    if 2 in PH:
      with ExitStack() as st:
        S2 = Sched(nc, sync_same_engine=SYNC_SAME)
        sb, ps = mk(st)
        KT = sb("KT", [128, 4, S], BF16)
        VV = sb("VV", [128, NB, 512], BF16)
        cst = sb("cst2", [128, 8, 128], BF16)
        qpool = Rot([sb(f"qt{i}", [128, 512], BF16) for i in range(3)])
        upool = Rot([sb(f"ue{i}", [128, 2, 512]) for i in range(3)])
        sppool = Rot([sb(f"sp{i}", [128, 2, 512], BF16) for i in range(4)])
        wpool = Rot([sb(f"ww{i}", [128, 2, 512], BF16) for i in range(4)])
        rspool = Rot([sb(f"rs{i}", [128, 2, 512], BF16) for i in range(4)])
        opool = Rot([sb(f"oo{i}", [64, 2, 512], BF16) for i in range(2)])
        PP = Rot([ps(f"PP{i}", F32, 1024) for i in range(3)])
        PO = [ps(f"PO{i}") for i in range(2)]
        negLI = cst.t[:, 1, :]
        negOnes = cst.t[:, 2, :]
        mask2 = cst.t[:, 6:8, :]

        S2.dma("pool", cst.t[:], consts_d, writes=[cst])
        for hp in range(4):
            S2.dma("sp", KT.t[:, hp, :], kT_d[hp * 128:(hp + 1) * 128, :], writes=[KT], par=True)
        for kb in range(NB):
            S2.dma("sp", VV.t[:, kb, :], v_d[kb * 128:(kb + 1) * 128, :], writes=[VV], par=True)

        units = []
        for g in range(NT):
            for hp in range(4):
                kbs = list(range(4 * g + 3, -1, -1))
                for idx, kb in enumerate(kbs):
                    units.append(dict(g=g, hp=hp, kb=kb, first=(idx == 0), last=(idx == len(kbs) - 1)))
        grp = {}

        def v3(t, lo, hi=512):
            return t.t[:].rearrange("p (h n) -> p h n", h=2)[:, :, lo:hi]

        def stA(u):
            g, hp, kb = u["g"], u["hp"], u["kb"]
            if u["first"]:
                q = qpool.next()
                S2.dma("sp", q.t[:], qT_d[hp * 128:(hp + 1) * 128, g * 512:(g + 1) * 512], writes=[q])
                rsa = rspool.next()
                rsb = rspool.next()
                S2.op("pool", lambda e: e.memset(rsa.t[:], 0.0), writes=[rsa])
                S2.op("pool", lambda e: e.memset(rsb.t[:], 0.0), writes=[rsb])
                grp[(g, hp)] = dict(q=q, rs=[rsa, rsb], n=0)
            G = grp[(g, hp)]
            u["G"] = G
            i = kb - 4 * g
            lo = max(i, 0) * 128
            u["lo"] = lo
            u["diag"] = i >= 0
            P = PP.next()
            u["P"] = P
            q = G["q"]
            for hh in range(2):
                p0, p1 = hh * 64, (hh + 1) * 64
                S2.op("pe", lambda e, hh=hh, p0=p0, p1=p1: e.matmul(P.t[:, hh * 512 + lo:(hh + 1) * 512], KT.t[p0:p1, hp, kb * 128:(kb + 1) * 128], q.t[p0:p1, lo:512],
                                                                    start=True, stop=True), reads=[KT, q], writes=[P])

        def stB1(u):
            P, lo = u["P"], u["lo"]
            ue = upool.next()
            u["ue"] = ue
            S2.op("act", lambda e: e.activation(out=ue.t[:, :, lo:512], in_=v3(P, lo), func=AF.Exp), reads=[P], writes=[ue])

        def stB2(u):
            lo, ue = u["lo"], u["ue"]
            sp = sppool.next()
            u["sp"] = sp
            S2.op("act", lambda e: e.activation(out=sp.t[:, :, lo:512], in_=ue.t[:, :, lo:512], func=AF.Ln, bias=1.0), reads=[ue], writes=[sp])
            if u["diag"]:
                S2.op("pool", lambda e: e.tensor_tensor(out=sp.t[:, :, lo:lo + 128], in0=sp.t[:, :, lo:lo + 128], in1=mask2, op=ALU.mult),
                      reads=[sp, cst], writes=[sp])

        def stC(u):
            P, lo, sp, G = u["P"], u["lo"], u["sp"], u["G"]
            rs = G["rs"][G["n"] % 2]
            rsn = G["rs"][(G["n"] + 1) % 2]
            G["n"] += 1
            first, last = u["first"], u["last"]
            for hh in range(2):
                S2.op("pe", lambda e, hh=hh: e.matmul(P.t[:, hh * 512 + lo:(hh + 1) * 512], negLI, sp.t[:, hh, lo:512], start=False, stop=first, skip_group_check=True),
                      reads=[cst, sp], writes=[P])
                if not first:
                    S2.op("pe", lambda e, hh=hh: e.matmul(P.t[:, hh * 512 + lo:(hh + 1) * 512], negOnes, rs.t[:, hh, lo:512], start=False, stop=True, skip_group_check=True),
                          reads=[cst, rs], writes=[P])
            if not last:
                S2.op("pool", lambda e: e.tensor_tensor(out=rsn.t[:, :, lo:512], in0=rs.t[:, :, lo:512], in1=sp.t[:, :, lo:512], op=ALU.add),
                      reads=[rs, sp], writes=[rsn])

        def stD(u):
            P, lo = u["P"], u["lo"]
            w = wpool.next()
            u["w"] = w
            S2.op("act", lambda e: e.activation(out=w.t[:, :, lo:512], in_=v3(P, lo), func=AF.Exp), reads=[P], writes=[w])
            if u["diag"]:
                S2.op("dve", lambda e: e.tensor_tensor(out=w.t[:, :, lo:lo + 128], in0=w.t[:, :, lo:lo + 128], in1=mask2, op=ALU.mult),
                      reads=[w, cst], writes=[w])

        def stE(u):
            w, lo, hp, kb, g = u["w"], u["lo"], u["hp"], u["kb"], u["g"]
            kw = dict(start=True, stop=True) if u["first"] else dict(start=False, stop=True, skip_group_check=True)
            for hh in range(2):
                h = hp * 2 + hh
                O = PO[hh]
                S2.op("pe", lambda e, O=O, hh=hh, h=h: e.matmul(O.t[0:64, lo:512], VV.t[:, kb, h * 64:(h + 1) * 64], w.t[:, hh, lo:512], **kw),
                      reads=[VV, w], writes=[O])
            if u["last"]:
                o = opool.next()
                for hh in range(2):
                    S2.op("dve", lambda e, hh=hh: e.tensor_copy(out=o.t[:, hh, :], in_=PO[hh].t[0:64, :]), reads=[PO[hh]], writes=[o])
                for hh in range(2):
                    h = hp * 2 + hh
                    S2.dma("sp", osbT_d[h * 64:(h + 1) * 64, g * 512:(g + 1) * 512], o.t[:, hh, :], reads=[o])

        nU = len(units)
        for s in range(nU + 3):
            if s < nU:
                stA(units[s])
            if 0 <= s - 1 < nU:
                stB1(units[s - 1])
                stB2(units[s - 1])
                stC(units[s - 1])
            if 0 <= s - 2 < nU:
                stD(units[s - 2])
            if 0 <= s - 3 < nU:
                stE(units[s - 3])
        S2.finalize()

    if 3 in PH:
      with ExitStack() as st:
        S3 = Sched(nc, sync_same_engine=SYNC_SAME)
        sb, ps = mk(st)
        wbr = sb("wbr", [128, 12, 1024], BF16)
        wout = sb("wout", [128, 8, 1024], BF16)
        wr32 = sb("wr32", [128, 8, 36])
        rbias = sb("rbias", [128, 36])
        cstf = sb("cstf", [128, 128])
        epsb = sb("epsb3", [128, 1])
        gtp = Rot([sb(f"gt{i}", [128, 3, 512], BF16) for i in range(10)])
        onp = Rot([sb(f"on{i}", [128, 12, 512], BF16) for i in range(2)])
        mTp = Rot([sb(f"mT{i}", [128, 8, 512], BF16) for i in range(2)])
        xpool = Rot([sb(f"x3_{i}", [128, 1024]) for i in range(3)])
        x1pool = Rot([sb(f"x1_{i}", [128, 1024]) for i in range(2)])
        h2pool = Rot([sb(f"h2_{i}", [128, 1024]) for i in range(3)])
        h2T32p = Rot([sb(f"h2T32_{i}", [128, 8, 128]) for i in range(3)])
        h2Tb = Rot([sb(f"h2Tb{i}", [128, 8, 512], BF16) for i in range(2)])
        junk = sb("junk3", [128, 1024])
        sspool = Rot([sb(f"ss3_{i}", [128, 4]) for i in range(3)])
        wk32 = Rot([sb(f"wk3_{i}", [128, 512]) for i in range(6)])
        rt = Rot([sb(f"rt{i}", [128, 256]) for i in range(3)])
        cbp = Rot([sb(f"cb{i}", [128, 32]) for i in range(3)])
        PM = Rot([ps(f"PM3_{i}") for i in range(4)])
        PTf = Rot([ps(f"PT3_{i}") for i in range(2)])
        PLp = Rot([ps(f"PL3_{i}") for i in range(2)])

        S3.op("pool", lambda e: e.memset(epsb.t[:], EPS), writes=[epsb])
        vecs3 = sb("vecs3", [128, NVEC])
        S3.dma("sp", vecs3.t[:], vecs_d, writes=[vecs3])
        S3.dma("sp", rbias.t[:], rbias_d, writes=[rbias])
        S3.dma("sp", cstf.t[:], consts_d[:, 0, :], writes=[cstf])
        for c in range(8):
            S3.dma("sp", wr32.t[:, c, :], wr_d[c * 128:(c + 1) * 128, :], writes=[wr32], par=True)
        for n in range(3):
            for c in range(4):
                S3.dma("pool", wbr.t[:, n * 4 + c, :], wbr_d[n, c * 128:(c + 1) * 128, :], writes=[wbr], par=True)
        for c in range(8):
            S3.dma("pool", wout.t[:, c, :], wout_d[c * 128:(c + 1) * 128, :], writes=[wout], par=True)

        def p3_merge(j, on, mT, gt):
            ms = []

            def one(n):
                U = PM.next()
                for c in range(4):
                    S3.op("pe", lambda e, c=c: e.matmul(U.t[:], wbr.t[:, n * 4 + c, j * 128:(j + 1) * 128], on.t[:, n * 4 + c, :], start=(c == 0), stop=(c == 3)),
                          reads=[wbr, on], writes=[U])
                m = wk32.next()
                S3.op("dve", lambda e: e.tensor_tensor(out=m.t[:], in0=U.t[:], in1=gt.t[:, n, :], op=ALU.mult), reads=[U, gt], writes=[m])
                ms.append(m)
            for n in range(3):
                one(n)
            a, b, c_ = ms
            S3.op("pool", lambda e: e.tensor_tensor(out=a.t[:], in0=a.t[:], in1=b.t[:], op=ALU.add), reads=[a, b], writes=[a])
            S3.op("pool", lambda e: e.tensor_tensor(out=mT.t[:, j, :], in0=a.t[:], in1=c_.t[:], op=ALU.add), reads=[a, c_], writes=[mT])

        def p3_route(blk, r0, PL):
            R = rt.next()
            lg = R.t[:, 0:36]

            def dv(fn, eng="dve"):
                S3.op(eng, fn, reads=[R], writes=[R])
            S3.op("dve", lambda e: e.tensor_tensor(out=lg, in0=PL.t[:, 0:36], in1=rbias.t[:], op=ALU.add), reads=[PL, rbias], writes=[R])
            gmax, ngmax, gsum, gprob = R.t[:, 40:41], R.t[:, 41:42], R.t[:, 42:43], R.t[:, 43:44]
            ge, goh, pen = R.t[:, 44:48], R.t[:, 48:52], R.t[:, 52:56]
            elm, oh1, elm2, oh2 = R.t[:, 64:96], R.t[:, 96:128], R.t[:, 128:160], R.t[:, 160:192]
            m1, m2, dd, ed, den, w1, w2, cw1, cw2 = [R.t[:, 200 + i:201 + i] for i in range(9)]
            t1 = R.t[:, 216:248]
            dv(lambda e: e.reduce_max(out=gmax, in_=R.t[:, 0:4], axis=AX.X))
            dv(lambda e: e.tensor_scalar(out=ngmax, in0=gmax, scalar1=-1.0, scalar2=None, op0=ALU.mult))
            dv(lambda e: e.activation(out=ge, in_=R.t[:, 0:4], func=AF.Exp, bias=ngmax, accum_out=gsum), eng="act")
            dv(lambda e: e.reciprocal(out=gprob, in_=gsum))
            dv(lambda e: e.tensor_scalar(out=goh, in0=R.t[:, 0:4], scalar1=gmax, scalar2=None, op0=ALU.is_equal))
            dv(lambda e: e.tensor_scalar(out=pen, in0=goh, scalar1=1.0, scalar2=BIG, op0=ALU.subtract, op1=ALU.mult))
            yield
            for gg in range(4):
                dv(lambda e, gg=gg: e.tensor_scalar(out=R.t[:, 64 + gg * 8:64 + (gg + 1) * 8], in0=R.t[:, 4 + gg * 8:4 + (gg + 1) * 8],
                                                    scalar1=R.t[:, 52 + gg:53 + gg], scalar2=None, op0=ALU.add))
            dv(lambda e: e.reduce_max(out=m1, in_=elm, axis=AX.X))
            dv(lambda e: e.tensor_scalar(out=oh1, in0=elm, scalar1=m1, scalar2=None, op0=ALU.is_equal))
            dv(lambda e: e.scalar_tensor_tensor(out=elm2, in0=oh1, scalar=-BIG, in1=elm, op0=ALU.mult, op1=ALU.add))
            yield
            dv(lambda e: e.reduce_max(out=m2, in_=elm2, axis=AX.X))
            dv(lambda e: e.tensor_scalar(out=oh2, in0=elm2, scalar1=m2, scalar2=None, op0=ALU.is_equal))
            dv(lambda e: e.tensor_tensor(out=dd, in0=m2, in1=m1, op=ALU.subtract))
            dv(lambda e: e.activation(out=ed, in_=dd, func=AF.Exp), eng="act")
            dv(lambda e: e.tensor_scalar(out=den, in0=ed, scalar1=1.0, scalar2=None, op0=ALU.add))
            dv(lambda e: e.reciprocal(out=w1, in_=den))
            yield
            dv(lambda e: e.tensor_tensor(out=w2, in0=ed, in1=w1, op=ALU.mult))
            dv(lambda e: e.tensor_tensor(out=cw1, in0=w1, in1=gprob, op=ALU.mult))
            dv(lambda e: e.tensor_tensor(out=cw2, in0=w2, in1=gprob, op=ALU.mult))
            dv(lambda e: e.tensor_scalar(out=t1, in0=oh1, scalar1=cw1, scalar2=None, op0=ALU.mult))
            cb = cbp.next()
            S3.op("dve", lambda e: e.scalar_tensor_tensor(out=cb.t[:], in0=oh2, scalar=cw2, in1=t1, op0=ALU.mult, op1=ALU.add), reads=[R], writes=[cb])
            S3.dma("sp", comb_d[r0:r0 + 128, :], cb.t[:], reads=[cb])

        def p3_xload(it, blk):
            r0 = it * 512 + blk * 128
            xt = xpool.next()
            S3.dma("sp", xt.t[:], x_d[r0:r0 + 128, :], writes=[xt])
            return xt

        def p3_outproj(it, blk, mT, sst, xt):
            r0 = it * 512 + blk * 128
            x1 = x1pool.next()

            def yhalf(half):
                Y = PM.next()
                for k in range(8):
                    S3.op("pe", lambda e, k=k: e.matmul(Y.t[:], mT.t[:, k, blk * 128:(blk + 1) * 128], wout.t[:, k, half * 512:(half + 1) * 512],
                                                        start=(k == 0), stop=(k == 7)), reads=[mT, wout], writes=[Y])
                S3.op("dve", lambda e: e.tensor_tensor(out=x1.t[:, half * 512:(half + 1) * 512], in0=Y.t[:], in1=xt.t[:, half * 512:(half + 1) * 512], op=ALU.add),
                      reads=[Y, xt], writes=[x1])
            for half in range(2):
                yhalf(half)
            S3.dma("sp", x1_d[r0:r0 + 128, :], x1.t[:], reads=[x1])
            S3.op("act", lambda e: e.activation(out=junk.t[:], in_=x1.t[:], func=AF.Square, accum_out=sst.t[:, blk:blk + 1]), reads=[x1], writes=[junk, sst])
            S3.op("act", lambda e: e.activation(out=sst.t[:, blk:blk + 1], in_=sst.t[:, blk:blk + 1], func=AF.Ln, scale=1.0 / 1024, bias=epsb.t[:, 0:1]), reads=[sst, epsb], writes=[sst])
            S3.op("act", lambda e: e.activation(out=sst.t[:, blk:blk + 1], in_=sst.t[:, blk:blk + 1], func=AF.Exp, scale=-0.5), reads=[sst], writes=[sst])
            h2 = h2pool.next()
            S3.op("act", lambda e: e.activation(out=h2.t[:], in_=x1.t[:], func=AF.Copy, scale=sst.t[:, blk:blk + 1]), reads=[x1, sst], writes=[h2])
            return dict(h2=h2, r0=r0, blk=blk)

        def p3_transposes(ctx, h2b):
            h2, blk = ctx["h2"], ctx["blk"]
            hT32 = h2T32p.next()
            ctx["hT32"] = hT32

            def thalf(half):
                pt = PTf.next()
                for c in range(4):
                    cc = half * 4 + c
                    S3.op("pe", lambda e, c=c, cc=cc: e.transpose(pt.t[:, c * 128:(c + 1) * 128], h2.t[:, cc * 128:(cc + 1) * 128], cstf.t[:]),
                          reads=[h2, cstf], writes=[pt])
                for c in range(4):
                    cc = half * 4 + c
                    S3.op("act", lambda e, c=c, cc=cc: e.activation(out=hT32.t[:, cc, :], in_=pt.t[:, c * 128:(c + 1) * 128], func=AF.Copy,
                                                                   scale=vecs3.t[:, V_GFFN + cc:V_GFFN + cc + 1]), reads=[pt, vecs3], writes=[hT32])
            for half in range(2):
                thalf(half)
            S3.op("pool", lambda e: e.tensor_copy(out=h2b.t[:, :, blk * 128:(blk + 1) * 128], in_=hT32.t[:]), reads=[hT32], writes=[h2b])

        def p3_router(ctx):
            hT32 = ctx["hT32"]
            PL = PLp.next()
            for k in range(8):
                S3.op("pe", lambda e, k=k: e.matmul(PL.t[:, 0:36], hT32.t[:, k, :], wr32.t[:, k, :], start=(k == 0), stop=(k == 7)),
                      reads=[hT32, wr32], writes=[PL])
            return p3_route(ctx["blk"], ctx["r0"], PL)

        def p3_loads(it):
            c0 = it * 512
            on = onp.next()
            for n, src in enumerate((osbT_d, olruT_d, oxT_d)):
                for c in range(4):
                    S3.dma("sp", on.t[:, n * 4 + c, :], src[c * 128:(c + 1) * 128, c0:c0 + 512], writes=[on], par=True)
            gts = []
            for j in range(8):
                gt = gtp.next()
                for n in range(3):
                    S3.dma("sp", gt.t[:, n, :], gT_d[(n * 8 + j) * 128:(n * 8 + j + 1) * 128, c0:c0 + 512], writes=[gt], par=True)
                gts.append(gt)
            return on, gts, mTp.next()

        cur = p3_loads(0)
        for j in range(8):
            p3_merge(j, cur[0], cur[2], cur[1][j])
        q1 = None
        q2 = None
        rgens = []

        def rstep():
            while rgens:
                try:
                    next(rgens[0])
                    return
                except StopIteration:
                    rgens.pop(0)

        def stage2(ctx):
            p3_transposes(ctx, ctx["h2b"])
            if ctx["blk"] == 3:
                c0_ = ctx["it"] * 512
                for c in range(8):
                    S3.dma("sp", h2T_d[c * 128:(c + 1) * 128, c0_:c0_ + 512], ctx["h2b"].t[:, c, :], reads=[ctx["h2b"]])

        allb = [(it, blk) for it in range(NT) for blk in range(4)]
        xts = {allb[0]: p3_xload(*allb[0])}
        for it in range(NT):
            nxt = p3_loads(it + 1) if it + 1 < NT else None
            sst = sspool.next()
            h2b = h2Tb.next()
            for blk in range(4):
                bi = it * 4 + blk
                if bi + 1 < len(allb):
                    xts[allb[bi + 1]] = p3_xload(*allb[bi + 1])
                ctx = p3_outproj(it, blk, cur[2], sst, xts.pop((it, blk)))
                ctx["h2b"] = h2b
                ctx["it"] = it
                rstep()
                if nxt is not None:
                    p3_merge(2 * blk, nxt[0], nxt[2], nxt[1][2 * blk])
                rstep()
                if q1 is not None:
                    stage2(q1)
                if nxt is not None:
                    p3_merge(2 * blk + 1, nxt[0], nxt[2], nxt[1][2 * blk + 1])
                rstep()
                if q2 is not None:
                    rgens.append(p3_router(q2))
                    rstep()
                q2 = q1
                q1 = ctx
            cur = nxt
        stage2(q1)
        if q2 is not None:
            rgens.append(p3_router(q2))
        rgens.append(p3_router(q1))
        while rgens:
            rstep()
        S3.finalize()

    if 4 in PH:
      ST = min(S, 2048)
      NST = S // ST
      NTT = ST // 256
      with ExitStack() as st:
        S4 = Sched(nc, sync_same_engine=SYNC_SAME)
        sb, ps = mk(st)
        h2T = sb("h2T4", [128, 8, ST], BF16)
        acc = sb("acc4", [128, ST // 128, 1024])
        accb = [Buf(f"acc{g}") for g in range(ST // 128)]
        cmb = sb("cmb4", [128, ST // 128, 32])
        sg_stage = sb("sgs", [128, 8, 256])
        su_stage = sb("sus", [128, 8, 256])
        sd_stage = sb("sds", [128, 2, 1024])
        wgp = Rot([sb(f"wg{i}", [128, 8, 256], BF16) for i in range(2)])
        wup = Rot([sb(f"wu{i}", [128, 8, 256], BF16) for i in range(2)])
        wdp = Rot([sb(f"wd{i}", [128, 2, 1024], BF16) for i in range(2)])
        sgp = Rot([sb(f"sg{i}", [128, 512]) for i in range(2)])
        actp = Rot([sb(f"at{i}", [128, 512], BF16) for i in range(3)])
        x1p = Rot([sb(f"x14_{i}", [128, 1024]) for i in range(4)])
        PG = Rot([ps(f"PG{i}") for i in range(2)])
        PU = Rot([ps(f"PU{i}") for i in range(2)])
        PD = Rot([ps(f"PD{i}") for i in range(4)])
        W = {}

        def load_w(ex):
            for c in range(8):
                S4.dma("sp", sg_stage.t[:, c, :], wg_d[ex, c * 128:(c + 1) * 128, :], writes=[sg_stage], par=True)
            for c in range(8):
                S4.dma("sp", su_stage.t[:, c, :], wu_d[ex, c * 128:(c + 1) * 128, :], writes=[su_stage], par=True)
            for c in range(2):
                S4.dma("sp", sd_stage.t[:, c, :], wd_d[ex, c * 128:(c + 1) * 128, :], writes=[sd_stage], par=True)

        def cast_w(key, which):
            pool_, stage = {"g": (wgp, sg_stage), "u": (wup, su_stage), "d": (wdp, sd_stage)}[which]
            wt = pool_.next()
            S4.op("act", lambda e: e.activation(out=wt.t[:], in_=stage.t[:], func=AF.Copy), reads=[stage], writes=[wt])
            W.setdefault(key, {})[which] = wt

        def gateup(t):
            ex, tc0 = t["ex"], t["j"] * 256
            wg, wu = W[t["key"]]["g"], W[t["key"]]["u"]
            Pg = PG.next()
            Pu = PU.next()
            for (Pq, wq) in ((Pg, wg), (Pu, wu)):
                for c2 in range(2):
                    for k in range(8):
                        S4.op("pe", lambda e, Pq=Pq, wq=wq, c2=c2, k=k: e.matmul(Pq.t[:, c2 * 256:(c2 + 1) * 256], wq.t[:, k, c2 * 128:(c2 + 1) * 128], h2T.t[:, k, tc0:tc0 + 256],
                                                                                 start=(k == 0), stop=(k == 7)), reads=[wq, h2T], writes=[Pq])
            sg = sgp.next()
            S4.op("act", lambda e: e.activation(out=sg.t[:], in_=Pg.t[:], func=AF.Silu), reads=[Pg], writes=[sg])
            at = actp.next()
            S4.op("dve", lambda e: e.tensor_tensor(out=at.t[:], in0=Pu.t[:], in1=sg.t[:], op=ALU.mult), reads=[Pu, sg], writes=[at])
            t["at"] = at

        def down(t):
            ex, tc0, at = t["ex"], t["j"] * 256, t["at"]
            wd = W[t["key"]]["d"]

            def one(blk, half):
                gb = (tc0 // 128) + blk
                Pd = PD.next()
                for c2 in range(2):
                    S4.op("pe", lambda e, c2=c2: e.matmul(Pd.t[:], at.t[:, c2 * 256 + blk * 128:c2 * 256 + (blk + 1) * 128],
                                                          wd.t[:, c2, half * 512:(half + 1) * 512], start=(c2 == 0), stop=(c2 == 1)),
                          reads=[at, wd], writes=[Pd])
                S4.op("dve", lambda e: e.scalar_tensor_tensor(out=acc.t[:, gb, half * 512:(half + 1) * 512], in0=Pd.t[:],
                                                             scalar=cmb.t[:, gb, ex:ex + 1], in1=acc.t[:, gb, half * 512:(half + 1) * 512],
                                                             op0=ALU.mult, op1=ALU.add), reads=[Pd, cmb, accb[gb]], writes=[accb[gb]])
            for blk in range(2):
                for half in range(2):
                    one(blk, half)

        X1 = {}

        def p4_x1load(t0, gb):
            r0 = t0 + gb * 128
            x1 = x1p.next()
            S4.dma("sp", x1.t[:], x1_d[r0:r0 + 128, :], writes=[x1])
            X1[(t0, gb)] = x1

        def p4_out(t0, gb):
            r0 = t0 + gb * 128
            if (t0, gb) not in X1:
                p4_x1load(t0, gb)
            x1 = X1.pop((t0, gb))
            S4.op("pool", lambda e: e.tensor_tensor(out=x1.t[:], in0=x1.t[:], in1=acc.t[:, gb, :], op=ALU.add), reads=[x1, accb[gb]], writes=[x1])
            S4.dma("sp", out_d[r0:r0 + 128, :], x1.t[:], reads=[x1])

        wsets = [(sti, ex) for sti in range(NST) for ex in range(n_exp)]
        load_w(wsets[0][1])
        for which in "gud":
            cast_w(wsets[0], which)
        for wi, (sti, ex) in enumerate(wsets):
            t0 = sti * ST
            if ex == 0:
                for c in range(8):
                    S4.dma("sp", h2T.t[:, c, :], h2T_d[c * 128:(c + 1) * 128, t0:t0 + ST], writes=[h2T], par=True)
                for gb in range(ST // 128):
                    S4.dma("sp", cmb.t[:, gb, :], comb_d[t0 + gb * 128:t0 + (gb + 1) * 128, :], writes=[cmb], par=True)
                for gb in range(ST // 128):
                    S4.op("dve", lambda e, gb=gb: e.memset(acc.t[:, gb, :], 0.0), writes=[accb[gb]])
            nxt = wsets[wi + 1] if wi + 1 < len(wsets) else None
            tiles = [dict(ex=ex, j=j, key=(sti, ex)) for j in range(NTT)]
            if ex == n_exp - 1:
                for gb in range(min(4, ST // 128)):
                    p4_x1load(t0, gb)
            prev = None
            for j, t in enumerate(tiles):
                if nxt is not None:
                    if j == 0:
                        load_w(nxt[1])
                    if j == 1 % NTT:
                        cast_w(nxt, "g")
                    if j == 2 % NTT:
                        cast_w(nxt, "u")
                    if j == 3 % NTT:
                        cast_w(nxt, "d")
                gateup(t)
                if prev is not None:
                    down(prev)
                prev = t
            down(prev)
            if ex == n_exp - 1:
                for gb in range(ST // 128):
                    p4_out(t0, gb)
        S4.finalize()
    return nc


def _host_consts():
    j = np.arange(128)[:, None]
    s = np.arange(128)[None, :]
    c = np.zeros((128, 8, 128), np.float32)
    c[:, 0, :] = np.eye(128, dtype=np.float32)
    c[:, 1, :] = np.where(j >= s, -1.0, 0.0)
    c[:, 2, :] = -1.0
    c[:, 3, :] = np.where(j < s, 1.0, 0.0)
    c[:, 4, :] = np.where((j // 64) == (s // 64), 1.0, 0.0)
    c[:, 5, :] = 1.0
    c[:, 6, :] = c[:, 3, :]
    c[:, 7, :] = c[:, 3, :]
    return c


def _shared_inputs(inp):
    f = np.float32
    vecs = np.zeros((128, NVEC), f)
    vecs[:, 0] = np.tile(inp["g_q_sb"][0], 2)
    vecs[:, 1] = np.tile(inp["g_k_sb"][0], 2)
    vecs[:, 2] = inp["g_q_x"][0]
    vecs[:, 3] = inp["g_k_x"][0]
    cw = inp["conv_w"][0]
    for c in range(4):
        for k in range(4):
            vecs[:, 4 + c * 4 + k] = cw[k, c * 128:(c + 1) * 128]
    vecs[:, 20:24] = inp["conv_b"][0].reshape(4, 128).T
    vecs[:, 24:28] = inp["lru_b_a"][0].reshape(4, 128).T
    vecs[:, 28:32] = inp["lru_b_i"][0].reshape(4, 128).T
    vecs[:, 32:36] = inp["lru_lambda"][0].reshape(4, 128).T
    vecs[:, 36:44] = inp["g_ffn"][0].reshape(8, 128).T
    gfull = np.zeros((128, 2, 1024), f)
    gfull[:, 0, :] = inp["g_mix"][0][None, :]
    gfull[:, 1, :] = inp["g_ffn"][0][None, :]
    gmemfull = np.ascontiguousarray(np.broadcast_to(inp["g_mem"][0][None, :], (128, 1024))).astype(f)
    rbias = np.ascontiguousarray(np.broadcast_to(np.concatenate([inp["b_group"][0], inp["b_expert"][0]])[None, :], (128, 36))).astype(f)
    wabd = np.zeros((128, 8, 128), f)
    for t, w in enumerate((inp["lru_w_a"][0], inp["lru_w_i"][0])):
        for c in range(4):
            for bb in range(2):
                wabd[bb * 64:(bb + 1) * 64, t * 4 + c, bb * 64:(bb + 1) * 64] = w[2 * c + bb]
    return {
        "w_in": np.ascontiguousarray(inp["w_in"][0]),
        "vecs": vecs, "gfull": gfull, "gmemfull": gmemfull, "rbias": rbias, "wabd": wabd,
        "w_mem_kv": np.ascontiguousarray(inp["w_mem_kv"][0]),
        "w_branch": np.ascontiguousarray(inp["w_branch"][0]),
        "w_out": np.ascontiguousarray(inp["w_out"][0]),
        "w_router": np.ascontiguousarray(np.concatenate([inp["w_group"][0], inp["w_expert"][0]], axis=1)),
        "w_gate": np.ascontiguousarray(inp["w_gate"][0].reshape(32, 1024, 256)),
        "w_up": np.ascontiguousarray(inp["w_up"][0].reshape(32, 1024, 256)),
        "w_down": np.ascontiguousarray(inp["w_down"][0].reshape(32, 256, 1024)),
        "consts": _host_consts(),
    }


def kernel(**inputs):
    inp = {k: np.asarray(v, dtype=np.float32) for k, v in inputs.items()}
    B, S, D = inp["x"].shape
    shared = _shared_inputs(inp)
    nc = build(S)
    in_maps = []
    for b in range(B):
        m = dict(shared)
        m["x"] = np.ascontiguousarray(inp["x"][b])
        m["mem"] = np.ascontiguousarray(inp["mem"][b])
        in_maps.append(m)
    res = run_bass_kernel_spmd(nc, in_maps, core_ids=list(range(B)))
    return np.stack([np.asarray(r["out"], dtype=np.float32) for r in res.results], axis=0)
```

```python
import contextlib
from contextlib import ExitStack
import numpy as np
import concourse.bass as bass
import concourse.mybir as mybir
from concourse.bass_utils import run_bass_kernel_spmd

F32 = mybir.dt.float32
BF16 = mybir.dt.bfloat16
AF = mybir.ActivationFunctionType
ALU = mybir.AluOpType
AX = mybir.AxisListType

SEM_CH = 30000
SYNC_SAME = True
N_DMA_SEMS = 6
EPS = 1e-6
BIG = 1.0e9


class Buf:
    __slots__ = ("name", "writes", "reads")

    def __init__(self, name=""):
        self.name = name
        self.writes = []
        self.reads = []


class TT:
    __slots__ = ("t", "b")

    def __init__(self, t, name=""):
        self.t = t
        self.b = Buf(name)


class Rot:
    def __init__(self, items):
        self.items = items
        self.i = 0

    def next(self):
        x = self.items[self.i % len(self.items)]
        self.i += 1
        return x


class SemPool:
    def __init__(self, nc, n):
        self.h = [nc.alloc_semaphore(name=f"gsem{i}") for i in range(n)]
        self.i = 0

    def take(self):
        h = self.h[self.i]
        self.i += 1
        return h

    def reset(self, nc):
        hs = self.h[:self.i]
        with nc.Block() as block:
            def body(g):
                for h in hs:
                    g.sem_clear(h)
            block.gpsimd(body)
        self.i = 0


class Sched:
    ENGS = ("pe", "act", "dve", "pool", "sp")

    def __init__(self, nc, sync_same_engine=True):
        self.nc = nc
        self.sempool = nc._sempool
        self.streams = {e: [] for e in self.ENGS}
        self.count = {e: 0 for e in self.ENGS}
        self.dma_count = {}
        self.sync_same = sync_same_engine
        self.dma_rr = {e: 0 for e in self.ENGS}
        self.waited = {}

    def _deps_for(self, reads, writes, par=False):
        deps = []
        for b in reads:
            deps.extend(b.writes)
        for b in writes:
            if not par:
                deps.extend(b.writes)
            deps.extend(b.reads)
        best = {}
        for d in deps:
            k = (d[0], d[1])
            if k not in best or best[k][2] < d[2]:
                best[k] = d
        return list(best.values())

    def _emit(self, eng, fn, deps, is_dma, dma_q=None):
        waits = []
        for d in deps:
            if d is None:
                continue
            if d[0] == "E":
                _, e2, n = d
                if e2 == eng and (eng == "pe" or (not self.sync_same and eng != "pool")) and not is_dma:
                    continue
                key = (eng, "E", e2)
            else:
                key = (eng, "D", d[1])
                n = d[2]
            if self.waited.get(key, 0) >= n:
                continue
            self.waited[key] = n
            waits.append(d)
        if is_dma:
            k = dma_q
            self.dma_count[k] = self.dma_count.get(k, 0) + 1
            tok = ("D", k, self.dma_count[k] * 16)
        else:
            self.count[eng] += 1
            tok = ("E", eng, self.count[eng])
        self.streams[eng].append((fn, waits, tok))
        return tok

    def _post(self, tok, reads, writes, par=False):
        for b in reads:
            b.reads.append(tok)
        for b in writes:
            if par and not b.reads:
                b.writes.append(tok)
            else:
                b.writes = [tok]
            b.reads = []

    def op(self, eng, fn, reads=(), writes=()):
        reads = [r.b if isinstance(r, TT) else r for r in reads]
        writes = [w.b if isinstance(w, TT) else w for w in writes]
        tok = self._emit(eng, fn, self._deps_for(reads, writes), False)
        self._post(tok, reads, writes)
        return tok

    def dma(self, eng, out, in_, reads=(), writes=(), par=False):
        reads = [r.b if isinstance(r, TT) else r for r in reads]
        writes = [w.b if isinstance(w, TT) else w for w in writes]
        deps = self._deps_for(reads, writes, par)
        base = {"sp": 0, "pool": N_DMA_SEMS, "act": 2 * N_DMA_SEMS}[eng]
        k = base + self.dma_rr[eng]
        self.dma_rr[eng] = (self.dma_rr[eng] + 1) % N_DMA_SEMS
        prev = self.dma_count.get(k, 0)
        if prev > 0:
            deps.append(("D", k, prev * 16))
        tok = self._emit(eng, lambda e, o=out, i=in_: e.dma_start(out=o, in_=i), deps, True, dma_q=k)
        self._post(tok, reads, writes, par)
        return tok

    def finalize(self, final_eng="sp"):
        nc = self.nc
        fin_waits = [("D", k, v * 16) for k, v in self.dma_count.items()]
        with ExitStack() as st:
            esems = {}
            for e in self.ENGS:
                n = (self.count[e] // SEM_CH) + 1
                esems[e] = [self.sempool.take() for i in range(n)]
            dsems = {k: self.sempool.take() for k in sorted(self.dma_count)}
            block = st.enter_context(nc.Block())

            def sem_for(tok):
                if tok[0] == "E":
                    _, e2, n = tok
                    idx = (n - 1) // SEM_CH
                    return esems[e2][idx], n - idx * SEM_CH
                return dsems[tok[1]], tok[2]

            def run(engname):
                def body(eng):
                    for fn, waits, tok in self.streams[engname]:
                        for w in waits:
                            s, v = sem_for(w)
                            eng.wait_ge(s, v)
                        ins = fn(eng)
                        s, v = sem_for(tok)
                        ins.then_inc(s, 1 if tok[0] == "E" else 16)
                    if engname == final_eng:
                        for w in fin_waits:
                            s, v = sem_for(w)
                            eng.wait_ge(s, v)
                return body

            block.tensor(run("pe"))
            block.scalar(run("act"))
            block.vector(run("dve"))
            block.gpsimd(run("pool"))
            block.sync(run("sp"))


NVEC = 8 * 3 + 4 + 16 + 4 * 4


def build(S, debug=False, phases=(0, 1, 2, 3, 4), n_exp=32):
    assert S % 512 == 0
    NT = S // 512
    NB = S // 128
    nc = bass.Bass("TRN2", target_bir_lowering=False)
    nc._sempool = SemPool(nc, 96)

    def din(name, shape):
        return nc.dram_tensor(name, shape, F32, kind="ExternalInput").ap()

    x_d = din("x", [S, 1024])
    mem_d = din("mem", [256, 1024])
    w_in_d = din("w_in", [1024, 6144])
    vecs_d = din("vecs", [128, NVEC])
    gfull_d = din("gfull", [128, 2, 1024])
    gmemfull_d = din("gmemfull", [128, 1024])
    rbias_d = din("rbias", [128, 36])
    wabd_d = din("wabd", [128, 8, 128])
    wkv_d = din("w_mem_kv", [1024, 1024])
    wbr_d = din("w_branch", [3, 512, 1024])
    wout_d = din("w_out", [1024, 1024])
    wr_d = din("w_router", [1024, 36])
    wg_d = din("w_gate", [32, 1024, 256])
    wu_d = din("w_up", [32, 1024, 256])
    wd_d = din("w_down", [32, 256, 1024])
    consts_d = din("consts", [128, 8, 128])
    out_d = nc.dram_tensor("out", [S, 1024], F32, kind="ExternalOutput").ap()

    skind = "ExternalOutput" if debug else "Internal"

    def dscr(name, shape, dt):
        return nc.dram_tensor(name, shape, dt, kind=skind).ap()

    qT_d = dscr("qT", [512, S], BF16)
    kT_d = dscr("kT", [512, S], BF16)
    v_d = dscr("v", [S, 512], BF16)
    olruT_d = dscr("olruT", [512, S], BF16)
    oxT_d = dscr("oxT", [512, S], BF16)
    gT_d = dscr("gT", [3072, S], BF16)
    osbT_d = dscr("osbT", [512, S], BF16)
    x1_d = dscr("x1", [S, 1024], F32)
    h2T_d = dscr("h2T", [1024, S], BF16)
    comb_d = dscr("comb", [S, 32], F32)

    V_GQSB, V_GKSB, V_GQX, V_GKX = 0, 1, 2, 3
    V_CONVW = 4
    V_CONVB = 20
    V_BA, V_BI, V_LAM = 24, 28, 32
    V_GFFN = 36

    PH = phases

    def mk(st):
        def sb(name, shape, dt=F32):
            return TT(st.enter_context(nc.sbuf_tensor("sb_" + name, shape, dt)), name)

        def ps(name, dt=F32, n=512):
            return TT(st.enter_context(nc.psum_tensor("ps_" + name, [128, n], dt)), name)
        return sb, ps

    def make_norm_helpers(SX, junk, sc, cst, wk16, wk32, PA):
        def rms_rstd(src_tt, src_ap, sstile, col, nfeat):
            SX.op("act", lambda e: e.activation(out=junk.t[:, 0:nfeat], in_=src_ap, func=AF.Square, accum_out=sstile.t[:, col:col + 1]),
                  reads=[src_tt], writes=[junk, sstile])
            SX.op("act", lambda e: e.activation(out=sstile.t[:, col:col + 1], in_=sstile.t[:, col:col + 1], func=AF.Ln, scale=1.0 / nfeat, bias=epsb.t[:, 0:1]),
                  reads=[sstile, epsb], writes=[sstile])
            SX.op("act", lambda e: e.activation(out=sstile.t[:, col:col + 1], in_=sstile.t[:, col:col + 1], func=AF.Exp, scale=-0.5),
                  reads=[sstile], writes=[sstile])

        def feat_norm_a(P, ncols):
            sq = wk16.next()
            SX.op("act", lambda e: e.activation(out=sq.t[:, 0:ncols], in_=P.t[:, 0:ncols], func=AF.Square), reads=[P], writes=[sq])
            return sq

        def feat_norm_b(P, sq, ncols, onesmat, nfeat, gcol, out_tt, out16):
            A = PA.next()
            SX.op("pe", lambda e: e.matmul(A.t[:, 0:ncols], onesmat, sq.t[:, 0:ncols], start=True, stop=True), reads=[cst, sq], writes=[A])
            ln = wk32.next()
            SX.op("act", lambda e: e.activation(out=ln.t[:, 0:ncols], in_=A.t[:, 0:ncols], func=AF.Ln, scale=1.0 / nfeat, bias=epsb.t[:, 0:1]), reads=[A, epsb], writes=[ln])
            r = wk32.next()
            SX.op("act", lambda e: e.activation(out=r.t[:, 0:ncols], in_=ln.t[:, 0:ncols], func=AF.Exp, scale=-0.5), reads=[ln], writes=[r])
            SX.op("dve", lambda e: e.scalar_tensor_tensor(out=out16, in0=P.t[:, 0:ncols], scalar=sc.t[:, gcol:gcol + 1], in1=r.t[:, 0:ncols],
                                                         op0=ALU.mult, op1=ALU.mult), reads=[P, sc, r], writes=[out_tt])

        def feat_norm(P, ncols, onesmat, nfeat, gcol, out_tt, out16):
            sq = feat_norm_a(P, ncols)
            feat_norm_b(P, sq, ncols, onesmat, nfeat, gcol, out_tt, out16)
        feat_norm.a = feat_norm_a
        feat_norm.b = feat_norm_b
        return rms_rstd, feat_norm

    with ExitStack() as stO:
        sbO, psO = mk(stO)
        vecs = sbO("vecs", [128, NVEC])
        cst = sbO("cst", [128, 8, 128], BF16)
        kmem = sbO("kmem", [128, 4, 256], BF16)
        vmem = sbO("vmem", [128, 2, 512], BF16)
        sc = sbO("sc", [128, 16])
        epsb = sbO("epsb", [128, 1])
        junk = sbO("junk", [128, 1024])
        ident = cst.t[:, 0, :]
        ones64 = cst.t[:, 4, :]
        ones128 = cst.t[:, 5, :]

        if 0 in PH:
          with ExitStack() as st:
            S0 = Sched(nc, sync_same_engine=SYNC_SAME)
            sb, ps = mk(st)
            gmemf = sb("gmemf", [128, 1024])
            wkv = sb("wkv", [128, 8, 1024], BF16)
            memT = sb("memT", [128, 8, 256], BF16)
            xpool = Rot([sb(f"xm{i}", [128, 1024]) for i in range(2)])
            hnpool = Rot([sb(f"hm{i}", [128, 1024], BF16) for i in range(2)])
            ssm = sb("ssm", [128, 4])
            wk32 = Rot([sb(f"wk0_{i}", [128, 512]) for i in range(4)])
            wk16 = Rot([sb(f"wb0_{i}", [128, 512], BF16) for i in range(2)])
            PT = Rot([ps(f"PT0_{i}", BF16, 1024) for i in range(2)])
            PM = Rot([ps(f"PM0_{i}") for i in range(4)])
            PA = Rot([ps(f"PA0_{i}") for i in range(2)])
            rms_rstd, feat_norm = make_norm_helpers(S0, junk, sc, cst, wk16, wk32, PA)

            S0.dma("sp", vecs.t[:], vecs_d, writes=[vecs])
            S0.dma("sp", gmemf.t[:], gmemfull_d, writes=[gmemf])
            S0.dma("pool", cst.t[:], consts_d, writes=[cst])
            for c in range(8):
                S0.dma("pool", wkv.t[:, c, :], wkv_d[c * 128:(c + 1) * 128, :], writes=[wkv], par=True)
            S0.op("pool", lambda e: e.memset(epsb.t[:], EPS), writes=[epsb])
            S0.op("dve", lambda e: e.tensor_scalar(out=sc.t[:, 0:1], in0=vecs.t[:, V_GQSB:V_GQSB + 1], scalar1=0.125, scalar2=None, op0=ALU.mult),
                  reads=[vecs], writes=[sc])
            S0.op("dve", lambda e: e.tensor_copy(out=sc.t[:, 1:2], in_=vecs.t[:, V_GKSB:V_GKSB + 1]), reads=[vecs], writes=[sc])
            S0.op("dve", lambda e: e.tensor_scalar(out=sc.t[:, 2:3], in0=vecs.t[:, V_GQX:V_GQX + 1], scalar1=float(128 ** -0.5), scalar2=None, op0=ALU.mult),
                  reads=[vecs], writes=[sc])
            S0.op("dve", lambda e: e.tensor_copy(out=sc.t[:, 3:4], in_=vecs.t[:, V_GKX:V_GKX + 1]), reads=[vecs], writes=[sc])
            S0.op("act", lambda e: e.activation(out=sc.t[:, 12:16], in_=vecs.t[:, V_LAM:V_LAM + 4], func=AF.Exp, scale=-1.0), reads=[vecs], writes=[sc])
            S0.op("act", lambda e: e.activation(out=sc.t[:, 12:16], in_=sc.t[:, 12:16], func=AF.Ln, bias=1.0), reads=[sc], writes=[sc])
            S0.op("dve", lambda e: e.tensor_scalar(out=sc.t[:, 4:8], in0=sc.t[:, 12:16], scalar1=-8.0, scalar2=None, op0=ALU.mult), reads=[sc], writes=[sc])
            S0.op("dve", lambda e: e.tensor_scalar(out=sc.t[:, 8:12], in0=sc.t[:, 12:16], scalar1=-16.0, scalar2=None, op0=ALU.mult), reads=[sc], writes=[sc])

            def p0_blk(blk):
                xt = xpool.next()
                S0.dma("sp", xt.t[:], mem_d[blk * 128:(blk + 1) * 128, :], writes=[xt])
                rms_rstd(xt, xt.t[:], ssm, blk, 1024)
                hn = hnpool.next()
                S0.op("dve", lambda e: e.scalar_tensor_tensor(out=hn.t[:], in0=xt.t[:], scalar=ssm.t[:, blk:blk + 1], in1=gmemf.t[:],
                                                             op0=ALU.mult, op1=ALU.mult), reads=[xt, ssm, gmemf], writes=[hn])
                pt = PT.next()
                for c in range(8):
                    S0.op("pe", lambda e, c=c: e.transpose(pt.t[:, c * 128:(c + 1) * 128], hn.t[:, c * 128:(c + 1) * 128], ident),
                          reads=[hn, cst], writes=[pt])
                S0.op("dve", lambda e: e.tensor_copy(out=memT.t[:, :, blk * 128:(blk + 1) * 128],
                                                     in_=pt.t[:].rearrange("p (c n) -> p c n", c=8)), reads=[pt], writes=[memT])
            for blk in range(2):
                p0_blk(blk)

            def p0_k(h):
                P = PM.next()
                for k in range(8):
                    S0.op("pe", lambda e, k=k: e.matmul(P.t[:, 0:256], wkv.t[:, k, h * 128:(h + 1) * 128], memT.t[:, k, :], start=(k == 0), stop=(k == 7)),
                          reads=[wkv, memT], writes=[P])
                feat_norm(P, 256, ones128, 128, 3, kmem, kmem.t[:, h, :])
            for h in range(4):
                p0_k(h)

            def p0_v(blk):
                P = PM.next()
                for k in range(8):
                    S0.op("pe", lambda e, k=k: e.matmul(P.t[:], memT.t[:, k, blk * 128:(blk + 1) * 128], wkv.t[:, k, 512:1024], start=(k == 0), stop=(k == 7)),
                          reads=[wkv, memT], writes=[P])
                S0.op("act", lambda e: e.activation(out=vmem.t[:, blk, :], in_=P.t[:], func=AF.Copy), reads=[P], writes=[vmem])
            for blk in range(2):
                p0_v(blk)
            S0.finalize()
            for t in (vecs, cst, kmem, vmem, sc, epsb, junk):
                t.b.writes = []
                t.b.reads = []

        if 1 in PH:
          with ExitStack() as st:
            S1 = Sched(nc, sync_same_engine=SYNC_SAME)
            sb, ps = mk(st)
            win = sb("win", [128, 8, 6144], BF16)
            gmixf = sb("gmixf", [128, 1024])
            wabd = sb("wabd", [128, 8, 128], BF16)
            xbuf = [sb(f"xbuf{c}", [128, 515]) for c in range(4)]
            state = sb("state", [128, 4])
            hT = Rot([sb(f"hT{i}", [128, 8, 512], BF16) for i in range(2)])
            xpool = Rot([sb(f"xt{i}", [128, 1024]) for i in range(3)])
            hnpool = Rot([sb(f"hn{i}", [128, 1024], BF16) for i in range(2)])
            sspool = Rot([sb(f"ss{i}", [128, 4]) for i in range(3)])
            wk32 = Rot([sb(f"wk{i}", [128, 512]) for i in range(10)])
            wk16 = Rot([sb(f"wb{i}", [128, 512], BF16) for i in range(6)])
            ob16 = Rot([sb(f"ob{i}", [128, 512], BF16) for i in range(6)])
            PT = Rot([ps(f"PT{i}", BF16, 1024) for i in range(1)])
            PM = Rot([ps(f"PM{i}") for i in range(5)])
            PA = Rot([ps(f"PA{i}") for i in range(2)])
            rms_rstd, feat_norm = make_norm_helpers(S1, junk, sc, cst, wk16, wk32, PA)

            S1.dma("sp", gmixf.t[:], gfull_d[:, 0, :], writes=[gmixf])
            S1.dma("pool", wabd.t[:], wabd_d, writes=[wabd])
            for c in range(8):
                for hf in range(2):
                    S1.dma("pool", win.t[:, c, hf * 3072:(hf + 1) * 3072], w_in_d[c * 128:(c + 1) * 128, hf * 3072:(hf + 1) * 3072], writes=[win], par=True)
            for c in range(4):
                S1.op("pool", lambda e, c=c: e.memset(xbuf[c].t[:, 0:3], 0.0), writes=[xbuf[c]])
            S1.op("pool", lambda e: e.memset(state.t[:], 0.0), writes=[state])

            def p1_xblk(it, blk, hTt, sst):
                r0 = it * 512 + blk * 128
                xt = xpool.next()
                S1.dma("sp", xt.t[:], x_d[r0:r0 + 128, :], writes=[xt])
                rms_rstd(xt, xt.t[:], sst, blk, 1024)
                hn = hnpool.next()
                S1.op("dve", lambda e: e.scalar_tensor_tensor(out=hn.t[:], in0=xt.t[:], scalar=sst.t[:, blk:blk + 1], in1=gmixf.t[:],
                                                             op0=ALU.mult, op1=ALU.mult), reads=[xt, sst, gmixf], writes=[hn])
                pt = PT.next()
                for c in range(8):
                    S1.op("pe", lambda e, c=c: e.transpose(pt.t[:, c * 128:(c + 1) * 128], hn.t[:, c * 128:(c + 1) * 128], ident),
                          reads=[hn, cst], writes=[pt])
                S1.op("dve", lambda e: e.tensor_copy(out=hTt.t[:, :, blk * 128:(blk + 1) * 128],
                                                     in_=pt.t[:].rearrange("p (c n) -> p c n", c=8)), reads=[pt], writes=[hTt])

            def proj_fm(ci, hTt):
                P = PM.next()
                for k in range(8):
                    S1.op("pe", lambda e, k=k: e.matmul(P.t[:], win.t[:, k, ci * 128:(ci + 1) * 128], hTt.t[:, k, :], start=(k == 0), stop=(k == 7)),
                          reads=[win, hTt], writes=[P])
                return P

            def p1_qk(ci, hTt, c0):
                L = {}

                def a():
                    L["P"] = proj_fm(ci, hTt)
                    L["sq"] = feat_norm.a(L["P"], 512)

                def b():
                    o = ob16.next()
                    feat_norm.b(L["P"], L["sq"], 512, ones64, 64, 0 if ci < 4 else 1, o, o.t[:])
                    dst = (qT_d if ci < 4 else kT_d)[(ci % 4) * 128:(ci % 4 + 1) * 128, c0:c0 + 512]
                    S1.dma("sp", dst, o.t[:], reads=[o])
                return [a, b]

            def p1_v(blk, hTt, c0):
                P = PM.next()
                for k in range(8):
                    S1.op("pe", lambda e, k=k: e.matmul(P.t[:], hTt.t[:, k, blk * 128:(blk + 1) * 128], win.t[:, k, 1024:1536], start=(k == 0), stop=(k == 7)),
                          reads=[win, hTt], writes=[P])
                o = ob16.next()
                S1.op("act", lambda e: e.activation(out=o.t[:], in_=P.t[:], func=AF.Copy), reads=[P], writes=[o])
                S1.dma("sp", v_d[c0 + blk * 128:c0 + (blk + 1) * 128, :], o.t[:], reads=[o])

            def p1_qx(h, hTt, c0):
                L = {}

                def a():
                    L["P"] = proj_fm(20 + h, hTt)
                    L["sq"] = feat_norm.a(L["P"], 512)

                def a2():
                    qn = wk16.next()
                    L["qn"] = qn
                    feat_norm.b(L["P"], L["sq"], 512, ones128, 128, 2, qn, qn.t[:])

                def b():
                    qn = L["qn"]
                    pms = []

                    def sx(mb):
                        Sx = PM.next()
                        S1.op("pe", lambda e: e.matmul(Sx.t[:], kmem.t[:, h, mb * 128:(mb + 1) * 128], qn.t[:], start=True, stop=True),
                              reads=[kmem, qn], writes=[Sx])
                        pm = wk16.next()
                        S1.op("act", lambda e: e.activation(out=pm.t[:], in_=Sx.t[:], func=AF.Exp), reads=[Sx], writes=[pm])
                        pms.append(pm)
                    for mb in range(2):
                        sx(mb)
                    L["pms"] = pms

                def c():
                    pms = L["pms"]
                    Ox = PM.next()
                    Dn = PA.next()
                    for mb in range(2):
                        S1.op("pe", lambda e, mb=mb: e.matmul(Ox.t[:], vmem.t[:, mb, h * 128:(h + 1) * 128], pms[mb].t[:], start=(mb == 0), stop=(mb == 1)),
                              reads=[vmem, pms[mb]], writes=[Ox])
                    for mb in range(2):
                        S1.op("pe", lambda e, mb=mb: e.matmul(Dn.t[:], ones128, pms[mb].t[:], start=(mb == 0), stop=(mb == 1)),
                              reads=[cst, pms[mb]], writes=[Dn])
                    rd = wk32.next()
                    S1.op("dve", lambda e: e.reciprocal(out=rd.t[:], in_=Dn.t[:]), reads=[Dn], writes=[rd])
                    o = ob16.next()
                    S1.op("dve", lambda e: e.tensor_tensor(out=o.t[:], in0=Ox.t[:], in1=rd.t[:], op=ALU.mult), reads=[Ox, rd], writes=[o])
                    S1.dma("sp", oxT_d[h * 128:(h + 1) * 128, c0:c0 + 512], o.t[:], reads=[o])
                return [a, a2, b, c]

            def p1_gate(j, hTt, c0):
                P = proj_fm(24 + j, hTt)
                o = ob16.next()
                S1.op("act", lambda e: e.activation(out=o.t[:], in_=P.t[:], func=AF.Sigmoid), reads=[P], writes=[o])
                S1.dma("sp", gT_d[j * 128:(j + 1) * 128, c0:c0 + 512], o.t[:], reads=[o])

            def lru_stages(c, hTt, c0):
                L = {}
                xb = xbuf[c]
                cw = V_CONVW + c * 4

                def s0():
                    Px = proj_fm(12 + c, hTt)
                    S1.op("act", lambda e: e.activation(out=xb.t[:, 3:515], in_=Px.t[:], func=AF.Copy), reads=[Px], writes=[xb])
                    Py = proj_fm(16 + c, hTt)
                    gy = wk32.next()
                    S1.op("act", lambda e: e.activation(out=gy.t[:], in_=Py.t[:], func=AF.Gelu_apprx_tanh), reads=[Py], writes=[gy])
                    L["gy"] = gy

                def s1():
                    xc = wk32.next()
                    L["xc"] = xc
                    S1.op("dve", lambda e: e.tensor_scalar(out=xc.t[:], in0=xb.t[:, 0:512], scalar1=vecs.t[:, cw:cw + 1],
                                                          scalar2=vecs.t[:, V_CONVB + c:V_CONVB + c + 1], op0=ALU.mult, op1=ALU.add),
                          reads=[xb, vecs], writes=[xc])
                    for k in range(1, 4):
                        S1.op("dve", lambda e, k=k: e.scalar_tensor_tensor(out=xc.t[:], in0=xb.t[:, k:k + 512], scalar=vecs.t[:, cw + k:cw + k + 1],
                                                                          in1=xc.t[:], op0=ALU.mult, op1=ALU.add),
                              reads=[xb, vecs, xc], writes=[xc])
                    S1.op("dve", lambda e: e.tensor_copy(out=xb.t[:, 0:3], in_=xb.t[:, 512:515]), reads=[xb], writes=[xb])
                    xcb = wk16.next()
                    L["xcb"] = xcb
                    S1.op("act", lambda e: e.activation(out=xcb.t[:], in_=xc.t[:], func=AF.Copy), reads=[xc], writes=[xcb])

                def s2():
                    xcb = L["xcb"]
                    Pr = PA.next()
                    S1.op("pe", lambda e: e.matmul(Pr.t[:], wabd.t[:, c, :], xcb.t[:], start=True, stop=True), reads=[wabd, xcb], writes=[Pr])
                    Pi = PA.next()
                    S1.op("pe", lambda e: e.matmul(Pi.t[:], wabd.t[:, 4 + c, :], xcb.t[:], start=True, stop=True), reads=[wabd, xcb], writes=[Pi])
                    rr = wk32.next()
                    S1.op("act", lambda e: e.activation(out=rr.t[:], in_=Pr.t[:], func=AF.Sigmoid, bias=vecs.t[:, V_BA + c:V_BA + c + 1]),
                          reads=[Pr, vecs], writes=[rr])
                    ii = wk32.next()
                    S1.op("act", lambda e: e.activation(out=ii.t[:], in_=Pi.t[:], func=AF.Sigmoid, bias=vecs.t[:, V_BI + c:V_BI + c + 1]),
                          reads=[Pi, vecs], writes=[ii])
                    L["rr"], L["ii"] = rr, ii

                def s3():
                    rr = L["rr"]
                    aa = wk32.next()
                    S1.op("act", lambda e: e.activation(out=aa.t[:], in_=rr.t[:], func=AF.Exp, scale=sc.t[:, 4 + c:5 + c]), reads=[rr, sc], writes=[aa])
                    a2 = wk32.next()
                    S1.op("act", lambda e: e.activation(out=a2.t[:], in_=rr.t[:], func=AF.Exp, scale=sc.t[:, 8 + c:9 + c]), reads=[rr, sc], writes=[a2])
                    S1.op("act", lambda e: e.activation(out=a2.t[:], in_=a2.t[:], func=AF.Ln, scale=-1.0, bias=1.0), reads=[a2], writes=[a2])
                    S1.op("act", lambda e: e.activation(out=a2.t[:], in_=a2.t[:], func=AF.Exp, scale=0.5), reads=[a2], writes=[a2])
                    L["aa"], L["a2"] = aa, a2

                def s4():
                    ii, xc, a2, aa = L["ii"], L["xc"], L["a2"], L["aa"]
                    S1.op("dve", lambda e: e.tensor_tensor(out=ii.t[:], in0=ii.t[:], in1=xc.t[:], op=ALU.mult), reads=[ii, xc], writes=[ii])
                    S1.op("dve", lambda e: e.tensor_tensor(out=ii.t[:], in0=ii.t[:], in1=a2.t[:], op=ALU.mult), reads=[ii, a2], writes=[ii])
                    hs = wk32.next()
                    L["hs"] = hs
                    S1.op("dve", lambda e: e.tensor_tensor_scan(out=hs.t[:], data0=aa.t[:], data1=ii.t[:], initial=state.t[:, c:c + 1],
                                                               op0=ALU.mult, op1=ALU.add), reads=[aa, ii, state], writes=[hs])
                    S1.op("dve", lambda e: e.tensor_copy(out=state.t[:, c:c + 1], in_=hs.t[:, 511:512]), reads=[hs], writes=[state])

                def s5():
                    hs, gy = L["hs"], L["gy"]
                    o = ob16.next()
                    S1.op("dve", lambda e: e.tensor_tensor(out=o.t[:], in0=hs.t[:], in1=gy.t[:], op=ALU.mult), reads=[hs, gy], writes=[o])
                    S1.dma("sp", olruT_d[c * 128:(c + 1) * 128, c0:c0 + 512], o.t[:], reads=[o])
                return [s0, s1, s2, s3, s4, s5]

            def p1_front(it):
                hTt = hT.next()
                sst = sspool.next()
                for blk in range(4):
                    p1_xblk(it, blk, hTt, sst)
                return hTt

            hT_next = p1_front(0)
            for it in range(NT):
                c0 = it * 512
                hTt = hT_next
                items = []
                for i4 in range(4):
                    items.append(p1_qx(i4, hTt, c0))
                    items.append(p1_qk(2 * i4, hTt, c0))
                    items.append(p1_qk(2 * i4 + 1, hTt, c0))
                    items.append([lambda blk=i4: p1_v(blk, hTt, c0)])
                active = []
                while items or active:
                    while items and len(active) < 3:
                        active.append(items.pop(0))
                    for itm in list(active):
                        itm.pop(0)()
                        if not itm:
                            active.remove(itm)
                stages = []
                for j in range(24):
                    if j % 6 == 0:
                        stages = lru_stages(j // 6, hTt, c0)
                    p1_gate(j, hTt, c0)
                    stages[j % 6]()
                    if j == 9 and it + 1 < NT:
                        hT_next = p1_front(it + 1)
            S1.finalize()

    if 2 in PH:
      with ExitStack() as st:
        S2 = Sched(nc, sync_same_engine=SYNC_SAME)
        sb, ps = mk(st)
        KT = sb("KT", [128, 4, S], BF16)
        VV = sb("VV", [128, NB, 512], BF16)
        cst = sb("cst2", [128, 8, 128], BF16)
        qpool = Rot([sb(f"qt{i}", [128, 512], BF16) for i in range(3)])
        upool = Rot([sb(f"ue{i}", [128, 2, 512]) for i in range(3)])
        sppool = Rot([sb(f"sp{i}", [128, 2, 512], BF16) for i in range(4)])
        wpool = Rot([sb(f"ww{i}", [128, 2, 512], BF16) for i in range(4)])
        rspool = Rot([sb(f"rs{i}", [128, 2, 512], BF16) for i in range(4)])
        opool = Rot([sb(f"oo{i}", [64, 2, 512], BF16) for i in range(2)])
        PP = Rot([ps(f"PP{i}", F32, 1024) for i in range(3)])
        PO = [ps(f"PO{i}") for i in range(2)]
        negLI = cst.t[:, 1, :]
        negOnes = cst.t[:, 2, :]
        mask2 = cst.t[:, 6:8, :]

        S2.dma("pool", cst.t[:], consts_d, writes=[cst])
        for hp in range(4):
            S2.dma("sp", KT.t[:, hp, :], kT_d[hp * 128:(hp + 1) * 128, :], writes=[KT], par=True)
        for kb in range(NB):
            S2.dma("sp", VV.t[:, kb, :], v_d[kb * 128:(kb + 1) * 128, :], writes=[VV], par=True)

        units = []
        for g in range(NT):
            for hp in range(4):
                kbs = list(range(4 * g + 3, -1, -1))
                for idx, kb in enumerate(kbs):
                    units.append(dict(g=g, hp=hp, kb=kb, first=(idx == 0), last=(idx == len(kbs) - 1)))
        grp = {}

        def v3(t, lo, hi=512):
            return t.t[:].rearrange("p (h n) -> p h n", h=2)[:, :, lo:hi]

        def stA(u):
            g, hp, kb = u["g"], u["hp"], u["kb"]
            if u["first"]:
                q = qpool.next()
                S2.dma("sp", q.t[:], qT_d[hp * 128:(hp + 1) * 128, g * 512:(g + 1) * 512], writes=[q])
                rsa = rspool.next()
                rsb = rspool.next()
                S2.op("dve", lambda e: e.memset(rsa.t[:], 0.0), writes=[rsa])
                S2.op("dve", lambda e: e.memset(rsb.t[:], 0.0), writes=[rsb])
                grp[(g, hp)] = dict(q=q, rs=[rsa, rsb], n=0)
            G = grp[(g, hp)]
            u["G"] = G
            i = kb - 4 * g
            lo = max(i, 0) * 128
            u["lo"] = lo
            u["diag"] = i >= 0
            P = PP.next()
            u["P"] = P
            q = G["q"]
            for hh in range(2):
                p0, p1 = hh * 64, (hh + 1) * 64
                S2.op("pe", lambda e, hh=hh, p0=p0, p1=p1: e.matmul(P.t[:, hh * 512 + lo:(hh + 1) * 512], KT.t[p0:p1, hp, kb * 128:(kb + 1) * 128], q.t[p0:p1, lo:512],
                                                                    start=True, stop=True), reads=[KT, q], writes=[P])

        def stB1(u):
            P, lo = u["P"], u["lo"]
            ue = upool.next()
            u["ue"] = ue
            S2.op("act", lambda e: e.activation(out=ue.t[:, :, lo:512], in_=v3(P, lo), func=AF.Exp), reads=[P], writes=[ue])

        def stB2(u):
            lo, ue = u["lo"], u["ue"]
            sp = sppool.next()
            u["sp"] = sp
            S2.op("act", lambda e: e.activation(out=sp.t[:, :, lo:512], in_=ue.t[:, :, lo:512], func=AF.Ln, bias=1.0), reads=[ue], writes=[sp])
            if u["diag"]:
                S2.op("dve", lambda e: e.tensor_tensor(out=sp.t[:, :, lo:lo + 128], in0=sp.t[:, :, lo:lo + 128], in1=mask2, op=ALU.mult),
                      reads=[sp, cst], writes=[sp])

        def stC(u):
            P, lo, sp, G = u["P"], u["lo"], u["sp"], u["G"]
            rs = G["rs"][G["n"] % 2]
            rsn = G["rs"][(G["n"] + 1) % 2]
            G["n"] += 1
            first, last = u["first"], u["last"]
            for hh in range(2):
                S2.op("pe", lambda e, hh=hh: e.matmul(P.t[:, hh * 512 + lo:(hh + 1) * 512], negLI, sp.t[:, hh, lo:512], start=False, stop=first, skip_group_check=True),
                      reads=[cst, sp], writes=[P])
                if not first:
                    S2.op("pe", lambda e, hh=hh: e.matmul(P.t[:, hh * 512 + lo:(hh + 1) * 512], negOnes, rs.t[:, hh, lo:512], start=False, stop=True, skip_group_check=True),
                          reads=[cst, rs], writes=[P])
            if not last:
                S2.op("pool", lambda e: e.tensor_tensor(out=rsn.t[:, :, lo:512], in0=rs.t[:, :, lo:512], in1=sp.t[:, :, lo:512], op=ALU.add),
                      reads=[rs, sp], writes=[rsn])

        def stD(u):
            P, lo = u["P"], u["lo"]
            w = wpool.next()
            u["w"] = w
            S2.op("act", lambda e: e.activation(out=w.t[:, :, lo:512], in_=v3(P, lo), func=AF.Exp), reads=[P], writes=[w])
            if u["diag"]:
                S2.op("dve", lambda e: e.tensor_tensor(out=w.t[:, :, lo:lo + 128], in0=w.t[:, :, lo:lo + 128], in1=mask2, op=ALU.mult),
                      reads=[w, cst], writes=[w])

        def stE(u):
            w, lo, hp, kb, g = u["w"], u["lo"], u["hp"], u["kb"], u["g"]
            kw = dict(start=True, stop=True) if u["first"] else dict(start=False, stop=True, skip_group_check=True)
            for hh in range(2):
                h = hp * 2 + hh
                O = PO[hh]
                S2.op("pe", lambda e, O=O, hh=hh, h=h: e.matmul(O.t[0:64, lo:512], VV.t[:, kb, h * 64:(h + 1) * 64], w.t[:, hh, lo:512], **kw),
                      reads=[VV, w], writes=[O])
            if u["last"]:
                o = opool.next()
                for hh in range(2):
                    S2.op("dve", lambda e, hh=hh: e.tensor_copy(out=o.t[:, hh, :], in_=PO[hh].t[0:64, :]), reads=[PO[hh]], writes=[o])
                for hh in range(2):
                    h = hp * 2 + hh
                    S2.dma("sp", osbT_d[h * 64:(h + 1) * 64, g * 512:(g + 1) * 512], o.t[:, hh, :], reads=[o])

        nU = len(units)
        for s in range(nU + 3):
            if s < nU:
                stA(units[s])
            if 0 <= s - 1 < nU:
                stB1(units[s - 1])
                stB2(units[s - 1])
                stC(units[s - 1])
            if 0 <= s - 2 < nU:
                stD(units[s - 2])
            if 0 <= s - 3 < nU:
                stE(units[s - 3])
        S2.finalize()

    if 3 in PH:
      with ExitStack() as st:
        S3 = Sched(nc, sync_same_engine=SYNC_SAME)
        sb, ps = mk(st)
        wbr = sb("wbr", [128, 12, 1024], BF16)
        wout = sb("wout", [128, 8, 1024], BF16)
        wr32 = sb("wr32", [128, 8, 36])
        rbias = sb("rbias", [128, 36])
        cstf = sb("cstf", [128, 128])
        epsb = sb("epsb3", [128, 1])
        gtp = Rot([sb(f"gt{i}", [128, 3, 512], BF16) for i in range(10)])
        onp = Rot([sb(f"on{i}", [128, 12, 512], BF16) for i in range(2)])
        mTp = Rot([sb(f"mT{i}", [128, 8, 512], BF16) for i in range(2)])
        xpool = Rot([sb(f"x3_{i}", [128, 1024]) for i in range(3)])
        x1pool = Rot([sb(f"x1_{i}", [128, 1024]) for i in range(2)])
        h2pool = Rot([sb(f"h2_{i}", [128, 1024]) for i in range(3)])
        h2T32p = Rot([sb(f"h2T32_{i}", [128, 8, 128]) for i in range(3)])
        h2Tb = Rot([sb(f"h2Tb{i}", [128, 8, 512], BF16) for i in range(2)])
        junk = sb("junk3", [128, 1024])
        sspool = Rot([sb(f"ss3_{i}", [128, 4]) for i in range(3)])
        wk32 = Rot([sb(f"wk3_{i}", [128, 512]) for i in range(6)])
        rt = Rot([sb(f"rt{i}", [128, 256]) for i in range(3)])
        cbp = Rot([sb(f"cb{i}", [128, 32]) for i in range(3)])
        PM = Rot([ps(f"PM3_{i}") for i in range(4)])
        PTf = Rot([ps(f"PT3_{i}") for i in range(2)])
        PLp = Rot([ps(f"PL3_{i}") for i in range(2)])

        S3.op("pool", lambda e: e.memset(epsb.t[:], EPS), writes=[epsb])
        vecs3 = sb("vecs3", [128, NVEC])
        S3.dma("sp", vecs3.t[:], vecs_d, writes=[vecs3])
        S3.dma("sp", rbias.t[:], rbias_d, writes=[rbias])
        S3.dma("sp", cstf.t[:], consts_d[:, 0, :], writes=[cstf])
        for c in range(8):
            S3.dma("sp", wr32.t[:, c, :], wr_d[c * 128:(c + 1) * 128, :], writes=[wr32], par=True)
        for n in range(3):
            for c in range(4):
                S3.dma("pool", wbr.t[:, n * 4 + c, :], wbr_d[n, c * 128:(c + 1) * 128, :], writes=[wbr], par=True)
        for c in range(8):
            S3.dma("pool", wout.t[:, c, :], wout_d[c * 128:(c + 1) * 128, :], writes=[wout], par=True)

        def p3_merge(j, on, mT, gt):
            ms = []

            def one(n):
                U = PM.next()
                for c in range(4):
                    S3.op("pe", lambda e, c=c: e.matmul(U.t[:], wbr.t[:, n * 4 + c, j * 128:(j + 1) * 128], on.t[:, n * 4 + c, :], start=(c == 0), stop=(c == 3)),
                          reads=[wbr, on], writes=[U])
                m = wk32.next()
                S3.op("dve", lambda e: e.tensor_tensor(out=m.t[:], in0=U.t[:], in1=gt.t[:, n, :], op=ALU.mult), reads=[U, gt], writes=[m])
                ms.append(m)
            for n in range(3):
                one(n)
            a, b, c_ = ms
            S3.op("pool", lambda e: e.tensor_tensor(out=a.t[:], in0=a.t[:], in1=b.t[:], op=ALU.add), reads=[a, b], writes=[a])
            S3.op("pool", lambda e: e.tensor_tensor(out=mT.t[:, j, :], in0=a.t[:], in1=c_.t[:], op=ALU.add), reads=[a, c_], writes=[mT])

        def p3_route(blk, r0, PL):
            R = rt.next()
            lg = R.t[:, 0:36]

            def dv(fn, eng="dve"):
                S3.op(eng, fn, reads=[R], writes=[R])
            S3.op("dve", lambda e: e.tensor_tensor(out=lg, in0=PL.t[:, 0:36], in1=rbias.t[:], op=ALU.add), reads=[PL, rbias], writes=[R])
            gmax, ngmax, gsum, gprob = R.t[:, 40:41], R.t[:, 41:42], R.t[:, 42:43], R.t[:, 43:44]
            ge, goh, pen = R.t[:, 44:48], R.t[:, 48:52], R.t[:, 52:56]
            elm, oh1, elm2, oh2 = R.t[:, 64:96], R.t[:, 96:128], R.t[:, 128:160], R.t[:, 160:192]
            m1, m2, dd, ed, den, w1, w2, cw1, cw2 = [R.t[:, 200 + i:201 + i] for i in range(9)]
            t1 = R.t[:, 216:248]
            dv(lambda e: e.reduce_max(out=gmax, in_=R.t[:, 0:4], axis=AX.X))
            dv(lambda e: e.tensor_scalar(out=ngmax, in0=gmax, scalar1=-1.0, scalar2=None, op0=ALU.mult))
            dv(lambda e: e.activation(out=ge, in_=R.t[:, 0:4], func=AF.Exp, bias=ngmax, accum_out=gsum), eng="act")
            dv(lambda e: e.reciprocal(out=gprob, in_=gsum))
            dv(lambda e: e.tensor_scalar(out=goh, in0=R.t[:, 0:4], scalar1=gmax, scalar2=None, op0=ALU.is_equal))
            dv(lambda e: e.tensor_scalar(out=pen, in0=goh, scalar1=1.0, scalar2=BIG, op0=ALU.subtract, op1=ALU.mult))
            yield
            for gg in range(4):
                dv(lambda e, gg=gg: e.tensor_scalar(out=R.t[:, 64 + gg * 8:64 + (gg + 1) * 8], in0=R.t[:, 4 + gg * 8:4 + (gg + 1) * 8],
                                                    scalar1=R.t[:, 52 + gg:53 + gg], scalar2=None, op0=ALU.add))
            dv(lambda e: e.reduce_max(out=m1, in_=elm, axis=AX.X))
            dv(lambda e: e.tensor_scalar(out=oh1, in0=elm, scalar1=m1, scalar2=None, op0=ALU.is_equal))
            dv(lambda e: e.scalar_tensor_tensor(out=elm2, in0=oh1, scalar=-BIG, in1=elm, op0=ALU.mult, op1=ALU.add))
            yield
            dv(lambda e: e.reduce_max(out=m2, in_=elm2, axis=AX.X))
            dv(lambda e: e.tensor_scalar(out=oh2, in0=elm2, scalar1=m2, scalar2=None, op0=ALU.is_equal))
            dv(lambda e: e.tensor_tensor(out=dd, in0=m2, in1=m1, op=ALU.subtract))
            dv(lambda e: e.activation(out=ed, in_=dd, func=AF.Exp), eng="act")
            dv(lambda e: e.tensor_scalar(out=den, in0=ed, scalar1=1.0, scalar2=None, op0=ALU.add))
            dv(lambda e: e.reciprocal(out=w1, in_=den))
            yield
            dv(lambda e: e.tensor_tensor(out=w2, in0=ed, in1=w1, op=ALU.mult))
            dv(lambda e: e.tensor_tensor(out=cw1, in0=w1, in1=gprob, op=ALU.mult))
            dv(lambda e: e.tensor_tensor(out=cw2, in0=w2, in1=gprob, op=ALU.mult))
            dv(lambda e: e.tensor_scalar(out=t1, in0=oh1, scalar1=cw1, scalar2=None, op0=ALU.mult))
            cb = cbp.next()
            S3.op("dve", lambda e: e.scalar_tensor_tensor(out=cb.t[:], in0=oh2, scalar=cw2, in1=t1, op0=ALU.mult, op1=ALU.add), reads=[R], writes=[cb])
            S3.dma("sp", comb_d[r0:r0 + 128, :], cb.t[:], reads=[cb])

        def p3_xload(it, blk):
            r0 = it * 512 + blk * 128
            xt = xpool.next()
            S3.dma("sp", xt.t[:], x_d[r0:r0 + 128, :], writes=[xt])
            return xt

        def p3_outproj(it, blk, mT, sst, xt):
            r0 = it * 512 + blk * 128
            x1 = x1pool.next()

            def yhalf(half):
                Y = PM.next()
                for k in range(8):
                    S3.op("pe", lambda e, k=k: e.matmul(Y.t[:], mT.t[:, k, blk * 128:(blk + 1) * 128], wout.t[:, k, half * 512:(half + 1) * 512],
                                                        start=(k == 0), stop=(k == 7)), reads=[mT, wout], writes=[Y])
                S3.op("dve", lambda e: e.tensor_tensor(out=x1.t[:, half * 512:(half + 1) * 512], in0=Y.t[:], in1=xt.t[:, half * 512:(half + 1) * 512], op=ALU.add),
                      reads=[Y, xt], writes=[x1])
            for half in range(2):
                yhalf(half)
            S3.dma("sp", x1_d[r0:r0 + 128, :], x1.t[:], reads=[x1])
            S3.op("act", lambda e: e.activation(out=junk.t[:], in_=x1.t[:], func=AF.Square, accum_out=sst.t[:, blk:blk + 1]), reads=[x1], writes=[junk, sst])
            S3.op("act", lambda e: e.activation(out=sst.t[:, blk:blk + 1], in_=sst.t[:, blk:blk + 1], func=AF.Ln, scale=1.0 / 1024, bias=epsb.t[:, 0:1]), reads=[sst, epsb], writes=[sst])
            S3.op("act", lambda e: e.activation(out=sst.t[:, blk:blk + 1], in_=sst.t[:, blk:blk + 1], func=AF.Exp, scale=-0.5), reads=[sst], writes=[sst])
            h2 = h2pool.next()
            S3.op("act", lambda e: e.activation(out=h2.t[:], in_=x1.t[:], func=AF.Copy, scale=sst.t[:, blk:blk + 1]), reads=[x1, sst], writes=[h2])
            return dict(h2=h2, r0=r0, blk=blk)

        def p3_transposes(ctx, h2b):
            h2, blk = ctx["h2"], ctx["blk"]
            hT32 = h2T32p.next()
            ctx["hT32"] = hT32

            def thalf(half):
                pt = PTf.next()
                for c in range(4):
                    cc = half * 4 + c
                    S3.op("pe", lambda e, c=c, cc=cc: e.transpose(pt.t[:, c * 128:(c + 1) * 128], h2.t[:, cc * 128:(cc + 1) * 128], cstf.t[:]),
                          reads=[h2, cstf], writes=[pt])
                for c in range(4):
                    cc = half * 4 + c
                    S3.op("act", lambda e, c=c, cc=cc: e.activation(out=hT32.t[:, cc, :], in_=pt.t[:, c * 128:(c + 1) * 128], func=AF.Copy,
                                                                   scale=vecs3.t[:, V_GFFN + cc:V_GFFN + cc + 1]), reads=[pt, vecs3], writes=[hT32])
            for half in range(2):
                thalf(half)
            S3.op("pool", lambda e: e.tensor_copy(out=h2b.t[:, :, blk * 128:(blk + 1) * 128], in_=hT32.t[:]), reads=[hT32], writes=[h2b])

        def p3_router(ctx):
            hT32 = ctx["hT32"]
            PL = PLp.next()
            for k in range(8):
                S3.op("pe", lambda e, k=k: e.matmul(PL.t[:, 0:36], hT32.t[:, k, :], wr32.t[:, k, :], start=(k == 0), stop=(k == 7)),
                      reads=[hT32, wr32], writes=[PL])
            return p3_route(ctx["blk"], ctx["r0"], PL)

        def p3_loads(it):
            c0 = it * 512
            on = onp.next()
            for n, src in enumerate((osbT_d, olruT_d, oxT_d)):
                for c in range(4):
                    S3.dma("sp", on.t[:, n * 4 + c, :], src[c * 128:(c + 1) * 128, c0:c0 + 512], writes=[on], par=True)
            gts = []
            for j in range(8):
                gt = gtp.next()
                for n in range(3):
                    S3.dma("sp", gt.t[:, n, :], gT_d[(n * 8 + j) * 128:(n * 8 + j + 1) * 128, c0:c0 + 512], writes=[gt], par=True)
                gts.append(gt)
            return on, gts, mTp.next()

        cur = p3_loads(0)
        for j in range(8):
            p3_merge(j, cur[0], cur[2], cur[1][j])
        q1 = None
        q2 = None
        rgens = []

        def rstep():
            while rgens:
                try:
                    next(rgens[0])
                    return
                except StopIteration:
                    rgens.pop(0)

        def stage2(ctx):
            p3_transposes(ctx, ctx["h2b"])
            if ctx["blk"] == 3:
                c0_ = ctx["it"] * 512
                for c in range(8):
                    S3.dma("sp", h2T_d[c * 128:(c + 1) * 128, c0_:c0_ + 512], ctx["h2b"].t[:, c, :], reads=[ctx["h2b"]])

        allb = [(it, blk) for it in range(NT) for blk in range(4)]
        xts = {allb[0]: p3_xload(*allb[0])}
        for it in range(NT):
            nxt = p3_loads(it + 1) if it + 1 < NT else None
            sst = sspool.next()
            h2b = h2Tb.next()
            for blk in range(4):
                bi = it * 4 + blk
                if bi + 1 < len(allb):
                    xts[allb[bi + 1]] = p3_xload(*allb[bi + 1])
                ctx = p3_outproj(it, blk, cur[2], sst, xts.pop((it, blk)))
                ctx["h2b"] = h2b
                ctx["it"] = it
                rstep()
                if nxt is not None:
                    p3_merge(2 * blk, nxt[0], nxt[2], nxt[1][2 * blk])
                rstep()
                if q1 is not None:
                    stage2(q1)
                if nxt is not None:
                    p3_merge(2 * blk + 1, nxt[0], nxt[2], nxt[1][2 * blk + 1])
                rstep()
                if q2 is not None:
                    rgens.append(p3_router(q2))
                    rstep()
                q2 = q1
                q1 = ctx
            cur = nxt
        stage2(q1)
        if q2 is not None:
            rgens.append(p3_router(q2))
        rgens.append(p3_router(q1))
        while rgens:
            rstep()
        S3.finalize()

    if 4 in PH:
      ST = min(S, 2048)
      NST = S // ST
      NTT = ST // 256
      with ExitStack() as st:
        S4 = Sched(nc, sync_same_engine=SYNC_SAME)
        sb, ps = mk(st)
        h2T = sb("h2T4", [128, 8, ST], BF16)
        acc = sb("acc4", [128, ST // 128, 1024])
        accb = [Buf(f"acc{g}") for g in range(ST // 128)]
        cmb = sb("cmb4", [128, ST // 128, 32])
        sg_stage = sb("sgs", [128, 8, 256])
        su_stage = sb("sus", [128, 8, 256])
        sd_stage = sb("sds", [128, 2, 1024])
        wgp = Rot([sb(f"wg{i}", [128, 8, 256], BF16) for i in range(2)])
        wup = Rot([sb(f"wu{i}", [128, 8, 256], BF16) for i in range(2)])
        wdp = Rot([sb(f"wd{i}", [128, 2, 1024], BF16) for i in range(2)])
        sgp = Rot([sb(f"sg{i}", [128, 512]) for i in range(2)])
        actp = Rot([sb(f"at{i}", [128, 512], BF16) for i in range(3)])
        x1p = Rot([sb(f"x14_{i}", [128, 1024]) for i in range(4)])
        PG = Rot([ps(f"PG{i}") for i in range(2)])
        PU = Rot([ps(f"PU{i}") for i in range(2)])
        PD = Rot([ps(f"PD{i}") for i in range(4)])
        W = {}

        def load_w(ex):
            for c in range(8):
                S4.dma("sp", sg_stage.t[:, c, :], wg_d[ex, c * 128:(c + 1) * 128, :], writes=[sg_stage], par=True)
            for c in range(8):
                S4.dma("sp", su_stage.t[:, c, :], wu_d[ex, c * 128:(c + 1) * 128, :], writes=[su_stage], par=True)
            for c in range(2):
                S4.dma("sp", sd_stage.t[:, c, :], wd_d[ex, c * 128:(c + 1) * 128, :], writes=[sd_stage], par=True)

        def cast_w(key, which):
            pool_, stage = {"g": (wgp, sg_stage), "u": (wup, su_stage), "d": (wdp, sd_stage)}[which]
            wt = pool_.next()
            S4.op("act", lambda e: e.activation(out=wt.t[:], in_=stage.t[:], func=AF.Copy), reads=[stage], writes=[wt])
            W.setdefault(key, {})[which] = wt

        def gateup(t):
            ex, tc0 = t["ex"], t["j"] * 256
            wg, wu = W[t["key"]]["g"], W[t["key"]]["u"]
            Pg = PG.next()
            Pu = PU.next()
            for (Pq, wq) in ((Pg, wg), (Pu, wu)):
                for c2 in range(2):
                    for k in range(8):
                        S4.op("pe", lambda e, Pq=Pq, wq=wq, c2=c2, k=k: e.matmul(Pq.t[:, c2 * 256:(c2 + 1) * 256], wq.t[:, k, c2 * 128:(c2 + 1) * 128], h2T.t[:, k, tc0:tc0 + 256],
                                                                                 start=(k == 0), stop=(k == 7)), reads=[wq, h2T], writes=[Pq])
            sg = sgp.next()
            S4.op("act", lambda e: e.activation(out=sg.t[:], in_=Pg.t[:], func=AF.Silu), reads=[Pg], writes=[sg])
            at = actp.next()
            S4.op("dve", lambda e: e.tensor_tensor(out=at.t[:], in0=Pu.t[:], in1=sg.t[:], op=ALU.mult), reads=[Pu, sg], writes=[at])
            t["at"] = at

        def down(t):
            ex, tc0, at = t["ex"], t["j"] * 256, t["at"]
            wd = W[t["key"]]["d"]

            def one(blk, half):
                gb = (tc0 // 128) + blk
                Pd = PD.next()
                for c2 in range(2):
                    S4.op("pe", lambda e, c2=c2: e.matmul(Pd.t[:], at.t[:, c2 * 256 + blk * 128:c2 * 256 + (blk + 1) * 128],
                                                          wd.t[:, c2, half * 512:(half + 1) * 512], start=(c2 == 0), stop=(c2 == 1)),
                          reads=[at, wd], writes=[Pd])
                S4.op("dve", lambda e: e.scalar_tensor_tensor(out=acc.t[:, gb, half * 512:(half + 1) * 512], in0=Pd.t[:],
                                                             scalar=cmb.t[:, gb, ex:ex + 1], in1=acc.t[:, gb, half * 512:(half + 1) * 512],
                                                             op0=ALU.mult, op1=ALU.add), reads=[Pd, cmb, accb[gb]], writes=[accb[gb]])
            for blk in range(2):
                for half in range(2):
                    one(blk, half)

        X1 = {}

        def p4_x1load(t0, gb):
            r0 = t0 + gb * 128
            x1 = x1p.next()
            S4.dma("sp", x1.t[:], x1_d[r0:r0 + 128, :], writes=[x1])
            X1[(t0, gb)] = x1

        def p4_out(t0, gb):
            r0 = t0 + gb * 128
            if (t0, gb) not in X1:
                p4_x1load(t0, gb)
            x1 = X1.pop((t0, gb))
            S4.op("pool", lambda e: e.tensor_tensor(out=x1.t[:], in0=x1.t[:], in1=acc.t[:, gb, :], op=ALU.add), reads=[x1, accb[gb]], writes=[x1])
            S4.dma("sp", out_d[r0:r0 + 128, :], x1.t[:], reads=[x1])

        wsets = [(sti, ex) for sti in range(NST) for ex in range(n_exp)]
        load_w(wsets[0][1])
        for which in "gud":
            cast_w(wsets[0], which)
        for wi, (sti, ex) in enumerate(wsets):
            t0 = sti * ST
            if ex == 0:
                for c in range(8):
                    S4.dma("sp", h2T.t[:, c, :], h2T_d[c * 128:(c + 1) * 128, t0:t0 + ST], writes=[h2T], par=True)
                for gb in range(ST // 128):
                    S4.dma("sp", cmb.t[:, gb, :], comb_d[t0 + gb * 128:t0 + (gb + 1) * 128, :], writes=[cmb], par=True)
                for gb in range(ST // 128):
                    S4.op("dve", lambda e, gb=gb: e.memset(acc.t[:, gb, :], 0.0), writes=[accb[gb]])
            nxt = wsets[wi + 1] if wi + 1 < len(wsets) else None
            tiles = [dict(ex=ex, j=j, key=(sti, ex)) for j in range(NTT)]
            if ex == n_exp - 1:
                for gb in range(min(4, ST // 128)):
                    p4_x1load(t0, gb)
            prev = None
            for j, t in enumerate(tiles):
                if nxt is not None:
                    if j == 0:
                        load_w(nxt[1])
                    if j == 1 % NTT:
                        cast_w(nxt, "g")
                    if j == 2 % NTT:
                        cast_w(nxt, "u")
                    if j == 3 % NTT:
                        cast_w(nxt, "d")
                gateup(t)
                if prev is not None:
                    down(prev)
                prev = t
            down(prev)
            if ex == n_exp - 1:
                for gb in range(ST // 128):
                    p4_out(t0, gb)
        S4.finalize()
    return nc


def _host_consts():
    j = np.arange(128)[:, None]
    s = np.arange(128)[None, :]
    c = np.zeros((128, 8, 128), np.float32)
    c[:, 0, :] = np.eye(128, dtype=np.float32)
    c[:, 1, :] = np.where(j >= s, -1.0, 0.0)
    c[:, 2, :] = -1.0
    c[:, 3, :] = np.where(j < s, 1.0, 0.0)
    c[:, 4, :] = np.where((j // 64) == (s // 64), 1.0, 0.0)
    c[:, 5, :] = 1.0
    c[:, 6, :] = c[:, 3, :]
    c[:, 7, :] = c[:, 3, :]
    return c


def _shared_inputs(inp):
    f = np.float32
    vecs = np.zeros((128, NVEC), f)
    vecs[:, 0] = np.tile(inp["g_q_sb"][0], 2)
    vecs[:, 1] = np.tile(inp["g_k_sb"][0], 2)
    vecs[:, 2] = inp["g_q_x"][0]
    vecs[:, 3] = inp["g_k_x"][0]
    cw = inp["conv_w"][0]
    for c in range(4):
        for k in range(4):
            vecs[:, 4 + c * 4 + k] = cw[k, c * 128:(c + 1) * 128]
    vecs[:, 20:24] = inp["conv_b"][0].reshape(4, 128).T
    vecs[:, 24:28] = inp["lru_b_a"][0].reshape(4, 128).T
    vecs[:, 28:32] = inp["lru_b_i"][0].reshape(4, 128).T
    vecs[:, 32:36] = inp["lru_lambda"][0].reshape(4, 128).T
    vecs[:, 36:44] = inp["g_ffn"][0].reshape(8, 128).T
    gfull = np.zeros((128, 2, 1024), f)
    gfull[:, 0, :] = inp["g_mix"][0][None, :]
    gfull[:, 1, :] = inp["g_ffn"][0][None, :]
    gmemfull = np.ascontiguousarray(np.broadcast_to(inp["g_mem"][0][None, :], (128, 1024))).astype(f)
    rbias = np.ascontiguousarray(np.broadcast_to(np.concatenate([inp["b_group"][0], inp["b_expert"][0]])[None, :], (128, 36))).astype(f)
    wabd = np.zeros((128, 8, 128), f)
    for t, w in enumerate((inp["lru_w_a"][0], inp["lru_w_i"][0])):
        for c in range(4):
            for bb in range(2):
                wabd[bb * 64:(bb + 1) * 64, t * 4 + c, bb * 64:(bb + 1) * 64] = w[2 * c + bb]
    return {
        "w_in": np.ascontiguousarray(inp["w_in"][0]),
        "vecs": vecs, "gfull": gfull, "gmemfull": gmemfull, "rbias": rbias, "wabd": wabd,
        "w_mem_kv": np.ascontiguousarray(inp["w_mem_kv"][0]),
        "w_branch": np.ascontiguousarray(inp["w_branch"][0]),
        "w_out": np.ascontiguousarray(inp["w_out"][0]),
        "w_router": np.ascontiguousarray(np.concatenate([inp["w_group"][0], inp["w_expert"][0]], axis=1)),
        "w_gate": np.ascontiguousarray(inp["w_gate"][0].reshape(32, 1024, 256)),
        "w_up": np.ascontiguousarray(inp["w_up"][0].reshape(32, 1024, 256)),
        "w_down": np.ascontiguousarray(inp["w_down"][0].reshape(32, 256, 1024)),
        "consts": _host_consts(),
    }


def kernel(**inputs):
    inp = {k: np.asarray(v, dtype=np.float32) for k, v in inputs.items()}
    B, S, D = inp["x"].shape
    shared = _shared_inputs(inp)
    nc = build(S)
    in_maps = []
    for b in range(B):
        m = dict(shared)
        m["x"] = np.ascontiguousarray(inp["x"][b])
        m["mem"] = np.ascontiguousarray(inp["mem"][b])
        in_maps.append(m)
    res = run_bass_kernel_spmd(nc, in_maps, core_ids=list(range(B)))
    return np.stack([np.asarray(r["out"], dtype=np.float32) for r in res.results], axis=0)
```
